# Optimizing a Trainium2 kernel written in Bass

```python
import jax, jax.numpy as jnp
from jax import lax
import numpy as np

D_MODEL = 1024
BATCH = 8
SEQ = 2048
DEPTH = 4

N_HEADS = 8
HEAD_DIM = D_MODEL // N_HEADS
MOBA_BLOCK = 256
MOBA_TOPK = 3
Q_CHUNK = 16
NEG_INF = -1e30
LRU_WIDTH = D_MODEL
LRU_BLOCKS = 8
LRU_BLOCK_DIM = LRU_WIDTH // LRU_BLOCKS
CONV_WIDTH = 4
LRU_C = 8.0
FFN_HIDDEN = -(-8 * D_MODEL // (3 * 256)) * 256
DN_ALPHA = (2 * DEPTH) ** 0.25
DN_BETA = (8 * DEPTH) ** -0.25
LN_EPS = 1e-5
N_ATTN_LAYERS = (DEPTH + 1) // 2
N_LRU_LAYERS = DEPTH // 2

kernel_name = "moba_rglru_deepnorm_hybrid"


def layer_norm(x, g, b):
    xf = x.astype(jnp.float32)
    mu = jnp.mean(xf, axis=-1, keepdims=True)
    var = jnp.mean(jnp.square(xf - mu), axis=-1, keepdims=True)
    return ((xf - mu) * lax.rsqrt(var + LN_EPS) * g.astype(jnp.float32) + b.astype(jnp.float32)).astype(x.dtype)


def alibi_slopes(n_heads):
    return jnp.exp2(-8.0 * (jnp.arange(n_heads, dtype=jnp.float32) + 1.0) / n_heads)


def moba_attention(x, w_qkv, w_o):
    B, S, _ = x.shape
    f32 = jnp.float32
    qkv = x @ w_qkv
    q, k, v = jnp.split(qkv, 3, axis=-1)
    to_heads = lambda t: t.reshape(B, S, N_HEADS, HEAD_DIM).transpose(0, 2, 1, 3)
    q, k, v = to_heads(q), to_heads(k), to_heads(v)

    n_blk = -(-S // MOBA_BLOCK)
    pad = n_blk * MOBA_BLOCK - S
    kb = jnp.pad(k, ((0, 0), (0, 0), (0, pad), (0, 0))).reshape(B, N_HEADS, n_blk, MOBA_BLOCK, HEAD_DIM)
    vb = jnp.pad(v, ((0, 0), (0, 0), (0, pad), (0, 0))).reshape(B, N_HEADS, n_blk, MOBA_BLOCK, HEAD_DIM)

    kmean = jnp.mean(kb.astype(f32), axis=3)
    pos = jnp.arange(S)
    q_blk = pos // MOBA_BLOCK
    gate = jnp.einsum('bhsd,bhnd->bhsn', q.astype(f32), kmean)
    past = jnp.arange(n_blk)[None, :] < q_blk[:, None]
    gate = jnp.where(past[None, None], gate, -jnp.inf)
    k_sel = min(MOBA_TOPK, n_blk)
    _, sel = lax.top_k(gate, k_sel)
    sel_valid = sel < q_blk[None, None, :, None]

    scale = HEAD_DIM ** -0.5
    slopes = alibi_slopes(N_HEADS)
    b_ix = jnp.arange(B)[:, None, None, None]
    h_ix = jnp.arange(N_HEADS)[None, :, None, None]
    offs = jnp.arange(MOBA_BLOCK)
    n_chunks = S // Q_CHUNK

    def chunk(c):
        t0 = c * Q_CHUNK
        qc = lax.dynamic_slice_in_dim(q, t0, Q_CHUNK, axis=2).astype(f32)
        sc = lax.dynamic_slice_in_dim(sel, t0, Q_CHUNK, axis=2)
        vc = lax.dynamic_slice_in_dim(sel_valid, t0, Q_CHUNK, axis=2)
        t = t0 + jnp.arange(Q_CHUNK)
        kg = kb[b_ix, h_ix, sc].astype(f32)
        vg = vb[b_ix, h_ix, sc].astype(f32)
        s_sel = jnp.einsum('bhqd,bhqnkd->bhqnk', qc, kg) * scale
        key_pos = sc[..., None] * MOBA_BLOCK + offs
        dist_sel = (t[None, None, :, None, None] - key_pos).astype(f32)
        s_sel = s_sel - slopes[None, :, None, None, None] * dist_sel
        s_sel = jnp.where(vc[..., None], s_sel, NEG_INF)
        own = t0 // MOBA_BLOCK
        ko = lax.dynamic_index_in_dim(kb, own, axis=2, keepdims=False).astype(f32)
        vo = lax.dynamic_index_in_dim(vb, own, axis=2, keepdims=False).astype(f32)
        dist_own = t[:, None] - (own * MOBA_BLOCK + offs)[None, :]
        s_own = jnp.einsum('bhqd,bhkd->bhqk', qc, ko) * scale
        s_own = jnp.where(dist_own[None, None] >= 0,
                          s_own - slopes[None, :, None, None] * dist_own.astype(f32)[None, None],
                          NEG_INF)
        scores = jnp.concatenate([s_sel.reshape(B, N_HEADS, Q_CHUNK, k_sel * MOBA_BLOCK), s_own], axis=-1)
        p = jax.nn.softmax(scores, axis=-1)
        p_sel = p[..., :k_sel * MOBA_BLOCK].reshape(B, N_HEADS, Q_CHUNK, k_sel, MOBA_BLOCK)
        p_own = p[..., k_sel * MOBA_BLOCK:]
        out = jnp.einsum('bhqnk,bhqnkd->bhqd', p_sel, vg) + jnp.einsum('bhqk,bhkd->bhqd', p_own, vo)
        return out.astype(x.dtype)

    o = lax.map(chunk, jnp.arange(n_chunks))
    o = o.transpose(1, 2, 0, 3, 4).reshape(B, N_HEADS, S, HEAD_DIM)
    o = o.transpose(0, 2, 1, 3).reshape(B, S, N_HEADS * HEAD_DIM)
    return o @ w_o


def rglru_block(x, w_in, conv_w, conv_b, w_a, b_a, w_x, b_x, lam, w_out):
    B, S, _ = x.shape
    f32 = jnp.float32
    xb, yb = jnp.split(x @ w_in, 2, axis=-1)
    gate_branch = jax.nn.gelu(yb)
    xp = jnp.pad(xb, ((0, 0), (CONV_WIDTH - 1, 0), (0, 0)))
    xc = sum(xp[:, tap:tap + S] * conv_w[tap] for tap in range(CONV_WIDTH)) + conv_b
    xg = xc.reshape(B, S, LRU_BLOCKS, LRU_BLOCK_DIM)
    r = jax.nn.sigmoid(jnp.einsum('bsgc,gcd->bsgd', xg, w_a).reshape(B, S, LRU_WIDTH) + b_a)
    i = jax.nn.sigmoid(jnp.einsum('bsgc,gcd->bsgd', xg, w_x).reshape(B, S, LRU_WIDTH) + b_x)
    log_a = -LRU_C * r.astype(f32) * jax.nn.softplus(-lam.astype(f32))
    a = jnp.exp(log_a)
    b_in = jnp.sqrt(-jnp.expm1(2.0 * log_a)) * (i * xc).astype(f32)

    def combine(left, right):
        a1, b1 = left
        a2, b2 = right
        return a1 * a2, a2 * b1 + b2

    _, h = lax.associative_scan(combine, (a, b_in), axis=1)
    return (h.astype(x.dtype) * gate_branch) @ w_out


def swiglu(x, w_in, w_out):
    g, u = jnp.split(x @ w_in, 2, axis=-1)
    return (jax.nn.silu(g) * u) @ w_out


def setup_inputs(seed: int = 0) -> dict:
    key = jax.random.key(seed)
    ks = jax.random.split(key, 20)
    f32 = jnp.float32
    D, DR, F, G, DG = D_MODEL, LRU_WIDTH, FFN_HIDDEN, LRU_BLOCKS, LRU_BLOCK_DIM
    nA, nR = N_ATTN_LAYERS, N_LRU_LAYERS
    nrm = lambda k, shape, s: jax.random.normal(k, shape, f32) * s
    x = nrm(ks[0], (BATCH, SEQ, D), 1.0)
    attn_w_qkv = nrm(ks[1], (nA, D, 3 * D), D ** -0.5)
    attn_w_o = nrm(ks[2], (nA, D, D), D ** -0.5 * DN_BETA)
    lru_w_in = nrm(ks[3], (nR, D, 2 * DR), D ** -0.5)
    lru_conv_w = nrm(ks[4], (nR, CONV_WIDTH, DR), CONV_WIDTH ** -0.5)
    lru_conv_b = nrm(ks[5], (nR, DR), 0.01)
    lru_w_a = nrm(ks[6], (nR, G, DG, DG), DG ** -0.5)
    lru_b_a = nrm(ks[7], (nR, DR), 0.01)
    lru_w_x = nrm(ks[8], (nR, G, DG, DG), DG ** -0.5)
    lru_b_x = nrm(ks[9], (nR, DR), 0.01)
    u = jax.random.uniform(ks[10], (nR, DR), f32, 0.9, 0.999)
    p = u ** (1.0 / LRU_C)
    lru_lambda = jnp.log(p) - jnp.log1p(-p)
    lru_w_out = nrm(ks[11], (nR, DR, D), DR ** -0.5 * DN_BETA)
    ffn_w_in = nrm(ks[12], (DEPTH, D, 2 * F), D ** -0.5)
    ffn_w_out = nrm(ks[13], (DEPTH, F, D), F ** -0.5 * DN_BETA)
    ln_g = 1.0 + nrm(ks[14], (DEPTH, 2, D), 0.02)
    ln_b = nrm(ks[15], (DEPTH, 2, D), 0.02)
    return {"x": x, "attn_w_qkv": attn_w_qkv, "attn_w_o": attn_w_o,
            "lru_w_in": lru_w_in, "lru_conv_w": lru_conv_w, "lru_conv_b": lru_conv_b,
            "lru_w_a": lru_w_a, "lru_b_a": lru_b_a, "lru_w_x": lru_w_x, "lru_b_x": lru_b_x,
            "lru_lambda": lru_lambda, "lru_w_out": lru_w_out,
            "ffn_w_in": ffn_w_in, "ffn_w_out": ffn_w_out, "ln_g": ln_g, "ln_b": ln_b}


def reference(x, attn_w_qkv, attn_w_o, lru_w_in, lru_conv_w, lru_conv_b, lru_w_a, lru_b_a,
              lru_w_x, lru_b_x, lru_lambda, lru_w_out, ffn_w_in, ffn_w_out, ln_g, ln_b):
    h = x
    for i in range(DEPTH):
        j = i // 2
        if i % 2 == 0:
            mix = moba_attention(h, attn_w_qkv[j], attn_w_o[j])
        else:
            mix = rglru_block(h, lru_w_in[j], lru_conv_w[j], lru_conv_b[j], lru_w_a[j], lru_b_a[j],
                              lru_w_x[j], lru_b_x[j], lru_lambda[j], lru_w_out[j])
        h = layer_norm(DN_ALPHA * h + mix, ln_g[i, 0], ln_b[i, 0])
        h = layer_norm(DN_ALPHA * h + swiglu(h, ffn_w_in[i], ffn_w_out[i]), ln_g[i, 1], ln_b[i, 1])
    return h
```

```python
import numpy as np
from contextlib import ExitStack
import concourse.bass as bass
import concourse.mybir as mybir
from concourse.bass_utils import run_bass_kernel_spmd

F32 = mybir.dt.float32
BF16 = mybir.dt.bfloat16
AF = mybir.ActivationFunctionType
ALU = mybir.AluOpType
AX = mybir.AxisListType

D = 1024
S = 2048
DEPTH = 4
NH = 8
FF = 2816
NF = FF // 128
ALPHA = float((2 * DEPTH) ** 0.25)
EPS = 1e-5
SCALE = float(128 ** -0.5)
NEG = -30000.0
ENGS = ["pe", "act", "dve", "pool", "sp"]

V_LNG = 0
V_LNB = 64
V_CW = 128
V_CB = 192
V_BA = 208
V_BX = 224
V_LAM = 240
NV = 256
C_ID = 0
C_BIAS = 128
C_PAST = 264
C_CAUS = 392
NCF = 904


class Prog:
    def __init__(self, same_engine_sync=False):
        self.ops = {e: [] for e in ENGS}
        self.cnt = {e: 0 for e in ENGS}
        self.dcnt = {}
        self.res = {}
        self.waited = {e: {} for e in ENGS}
        self.same_engine_sync = same_engine_sync
        self.barrier = {}
        self.early = {}
        self.early_next = None
        self.default_early = False

    def _counts(self):
        return {"e_" + e: self.cnt[e] for e in ENGS if self.cnt[e] > 0}

    def snapshot_early(self):
        self.early_next = self._counts()

    def phase_barrier(self):
        self.barrier = self._counts()
        self.early = self.early_next if self.early_next is not None else self.barrier
        self.early_next = None

    def _st(self, key):
        st = self.res.get(key)
        if st is None:
            st = {"w": None, "r": {}}
            self.res[key] = st
        return st

    def op(self, eng, fn, reads=(), writes=(), dma=None, inc=True, nobarrier=False, ss=False):
        need = {} if nobarrier else dict(self.early if self.default_early else self.barrier)

        def add(tok):
            if tok is None:
                return
            s, v = tok
            if need.get(s, 0) < v:
                need[s] = v

        for r in reads:
            add(self._st(r)["w"])
        for w in writes:
            st = self._st(w)
            add(st["w"])
            for s, v in st["r"].items():
                add((s, v))
        if dma is None:
            if inc:
                self.cnt[eng] += 1
                tok = ("e_" + eng, self.cnt[eng])
                inc = 1
            else:
                tok = ("e_" + eng, self.cnt[eng] + 1)
                inc = 0
        else:
            self.dcnt[dma] = self.dcnt.get(dma, 0) + 16
            tok = (dma, self.dcnt[dma])
            inc = 16
        waits = []
        wd = self.waited[eng]
        for s, v in need.items():
            if s == "e_" + eng and (eng == "pe" or not (self.same_engine_sync or ss)):
                continue
            if wd.get(s, 0) >= v:
                continue
            wd[s] = v
            waits.append((s, v))
        self.ops[eng].append((fn, waits, tok[0], inc))
        for r in reads:
            st = self._st(r)
            if st["r"].get(tok[0], 0) < tok[1]:
                st["r"][tok[0]] = tok[1]
        for w in writes:
            st = self._st(w)
            st["w"] = tok
            st["r"] = {}
        return tok

    def final_waits(self, eng, toks):
        self.ops[eng].append((None, list(toks), None, 0))

    def emit(self, nc, es):
        sems = {}
        names = ["e_" + e for e in ENGS if self.cnt[e] > 0] + list(self.dcnt.keys())
        for n in names:
            sems[n] = es.enter_context(nc.semaphore(n))
        block = es.enter_context(nc.Block())
        engmap = {"pe": block.tensor, "act": block.scalar, "dve": block.vector,
                  "pool": block.gpsimd, "sp": block.sync}
        for e in ENGS:
            ops = self.ops[e]
            if not ops:
                continue

            def body(eng, ops=ops):
                for fn, waits, sname, inc in ops:
                    for s, v in waits:
                        eng.wait_ge(sems[s], v)
                    if fn is not None:
                        ins = fn(eng)
                        if inc:
                            ins.then_inc(sems[sname], inc)

            engmap[e](body)


class Ring:
    def __init__(self, n):
        self.n = n
        self.i = -1

    def next(self):
        self.i = (self.i + 1) % self.n
        return self.i


class K:
    NSLOT = 3
    SLOT_ELEMS = 3072
    ARENA_BYTES = 85 * 1024

    def __init__(self, phases, dbg=None):
        self.phases = phases
        self.dbg = dbg
        self.nc = nc = bass.Bass("TRN2", target_bir_lowering=False)
        self.P = Prog()
        dt = lambda name, shape, kind="ExternalInput": nc.dram_tensor(name, shape, F32, kind=kind).ap()
        self.xT = dt("xT", [D, S])
        self.outT = dt("outT", [D, S], "ExternalOutput")
        self.wqkv = dt("wqkv", [2, NH, 128, 3072])
        self.wo = dt("wo", [2, 8, 128, 1024])
        self.lwin = dt("lwin", [2, 8, 128, 2048])
        self.lwout = dt("lwout", [2, 8, 128, 1024])
        self.lwa = dt("lwa", [2, 128, 1024])
        self.lwx = dt("lwx", [2, 128, 1024])
        self.fwin = dt("fwin", [DEPTH, NF, 128, 2048])
        self.fwout = dt("fwout", [DEPTH, 8, 128, FF])
        self.vecs_d = dt("vecs", [128, NV])
        self.cf_d = dt("cf", [128, NCF])
        self.e8_d = dt("e8", [8, 1024])
        if dbg:
            self.dbgT = dt("dbgT", [D, S], "ExternalOutput")

    def view(self, off, shape, dtype):
        nel = int(np.prod(shape[1:]))
        nbytes = nel * (4 if dtype == F32 else 2)
        assert off % 4 == 0 and off + nbytes <= self.ARENA_BYTES, (off, nbytes)
        a = self.arena[0:shape[0], off // 4:(off + nbytes + 3) // 4]
        if dtype != F32:
            a = a.bitcast(dtype)
        if len(shape) == 3:
            a = a.rearrange("p (a b) -> p a b", b=shape[2])
        elif len(shape) == 4:
            a = a.rearrange("p (a b c) -> p a b c", b=shape[2], c=shape[3])
        return a

    def wload(self, src, nel):
        s = self.wring_i.next()
        dst = self.wring[:, s, 0:nel]
        key = ("w", s)
        self.P.op("pool", lambda e: e.dma_start(out=dst, in_=src, max_dma_last_dim=4096),
                  writes=[key], dma="dw%d" % s, nobarrier=True)
        return dst, key

    def mm(self, out, lhsT, rhs, start, stop, reads, writes, inc=None, sgc=False):
        if inc is None:
            inc = stop
        self.P.op("pe", lambda e: e.matmul(out, lhsT=lhsT, rhs=rhs, start=start, stop=stop, skip_group_check=sgc),
                  reads=reads, writes=writes, inc=inc)

    def act(self, out, in_, func, reads, writes, bias=None, scale=None, ss=False):
        kw = {}
        if bias is not None:
            kw["bias"] = bias
        if scale is not None:
            kw["scale"] = scale
        self.P.op("act", lambda e: e.activation(out=out, in_=in_, func=func, **kw), reads=reads, writes=writes, ss=ss)

    def tt(self, out, in0, in1, op, reads, writes, eng="dve", ss=False):
        self.P.op(eng, lambda e: e.tensor_tensor(out=out, in0=in0, in1=in1, op=op), reads=reads, writes=writes, ss=ss)

    def ts(self, out, in0, s1, s2, op0, op1, reads, writes, eng="dve", ss=False):
        if op1 is None:
            self.P.op(eng, lambda e: e.tensor_scalar(out=out, in0=in0, scalar1=s1, scalar2=None, op0=op0),
                      reads=reads, writes=writes, ss=ss)
        else:
            self.P.op(eng, lambda e: e.tensor_scalar(out=out, in0=in0, scalar1=s1, scalar2=s2, op0=op0, op1=op1),
                      reads=reads, writes=writes, ss=ss)

    def stt(self, out, in0, scalar, in1, op0, op1, reads, writes, ss=False):
        self.P.op("dve", lambda e: e.scalar_tensor_tensor(out=out, in0=in0, scalar=scalar, in1=in1, op0=op0, op1=op1),
                  reads=reads, writes=writes, ss=ss)

    @staticmethod
    def bk(b):
        return [("ps", b)]

    def vcol(self, col):
        return self.vec[:, col:col + 1]

    def build(self):
        nc, P = self.nc, self.P
        with ExitStack() as es:
            sb = lambda name, shape, dtype: es.enter_context(nc.sbuf_tensor(name, shape, dtype))
            self.hT32 = sb("hT32", [128, 8, S], F32)
            self.hTb = sb("hTb", [128, 8, S], BF16)
            self.vec = sb("vec_sb", [128, NV], F32)
            self.dv = sb("dv", [128, 64], F32)
            self.dv2 = sb("dv2", [128, 64], F32)
            self.cf = sb("cf_sb", [128, NCF], F32)
            self.identb = sb("identb", [128, 128], BF16)
            self.onesb = sb("onesb", [128, 128], BF16)
            self.causb = sb("causb", [128, 2, 256], BF16)
            self.e8b = sb("e8b", [8, 8, 128], BF16)
            self.cpow = sb("cpow", [128, 2], F32)
            self.wring = sb("wring", [128, self.NSLOT, self.SLOT_ELEMS], BF16)
            self.wring_i = Ring(self.NSLOT)
            self.zring, self.tring, self.ybank = Ring(3), Ring(4), Ring(2)
            self.arena = sb("arena", [128, self.ARENA_BYTES // 4], F32)
            self.ps = [es.enter_context(nc.psum_tensor("ps%d" % i, [128, 512], F32)) for i in range(8)]

            P.same_engine_sync = True
            self.setup()
            for ph in self.phases:
                kind, li = ph
                P.phase_barrier()
                if kind == "attn":
                    self.attn(li)
                elif kind == "lru":
                    self.lru(li)
                elif kind == "ffn":
                    self.ffn(li)
            ov = self.outT.rearrange("(c p) t -> p c t", p=128)
            toks = []
            for t in range(4):
                toks.append(P.op("sp", lambda e, t=t: e.dma_start(out=ov[:, :, t * 512:(t + 1) * 512], in_=self.hT32[:, :, t * 512:(t + 1) * 512]),
                                 reads=[("h32", c, t) for c in range(8)], dma="dout%d" % t))
            P.final_waits("sp", toks)
            P.emit(nc, es)
        return nc

    def setup(self):
        P = self.P
        h32keys = [("h32", c, t) for c in range(8) for t in range(4)]
        hbkeys = [("hb", c, t) for c in range(8) for t in range(4)]
        P.op("sp", lambda e: e.dma_start(out=self.vec[:], in_=self.vecs_d), writes=["cst"], dma="dc")
        P.op("sp", lambda e: e.dma_start(out=self.cf[:], in_=self.cf_d), writes=["cst"], dma="dc")
        P.op("pool", lambda e: e.dma_start(out=self.e8b[:].rearrange("p a b -> p (a b)"), in_=self.e8_d), writes=["cb"], dma="dg0")
        xv = self.xT.rearrange("(c p) t -> p c t", p=128)
        for t in range(4):
            P.op("sp", lambda e, t=t: e.dma_start(out=self.hT32[:, :, t * 512:(t + 1) * 512], in_=xv[:, :, t * 512:(t + 1) * 512]),
                 writes=[("h32", c, t) for c in range(8)], dma="dx%d" % t)
        for t in range(4):
            for c in range(8):
                tsl = slice(t * 512, (t + 1) * 512)
                if c % 2 == 0:
                    P.op("dve", lambda e, c=c, tsl=tsl: e.tensor_copy(self.hTb[:, c, tsl], self.hT32[:, c, tsl]),
                         reads=[("h32", c, t)], writes=[("hb", c, t)])
                else:
                    P.op("act", lambda e, c=c, tsl=tsl: e.copy(self.hTb[:, c, tsl], self.hT32[:, c, tsl]),
                         reads=[("h32", c, t)], writes=[("hb", c, t)])
        P.op("dve", lambda e: e.tensor_copy(self.identb[:], self.cf[:, C_ID:C_ID + 128]), reads=["cst"], writes=["cb"])
        P.op("dve", lambda e: e.tensor_copy(self.causb[:].rearrange("p a b -> p (a b)"), self.cf[:, C_CAUS:C_CAUS + 512]),
             reads=["cst"], writes=["cb"])
        P.op("dve", lambda e: e.memset(self.onesb[:], 1.0), writes=["cb"])
        P.op("dve", lambda e: e.memset(self.cpow[:, 0:1], -0.5), writes=["cb"])
        P.op("dve", lambda e: e.memset(self.cpow[:, 1:2], 0.5), writes=["cb"])
        lam = self.vec[:, V_LAM:V_LAM + 16]
        e_, z_, z2_, q_ = (self.dv2[:, i * 16:(i + 1) * 16] for i in range(4))
        self.act(e_, lam, AF.Exp, ["cst"], ["dv"], scale=-1.0)
        self.ts(z_, e_, 2.0, None, ALU.add, None, ["dv"], ["dv"])
        P.op("dve", lambda e: e.reciprocal(z_, z_), reads=["dv"], writes=["dv"])
        self.tt(z_, z_, e_, ALU.mult, ["dv"], ["dv"])
        self.tt(z2_, z_, z_, ALU.mult, ["dv"], ["dv"])
        self.ts(q_, z2_, 1.0 / 13.0, None, ALU.mult, None, ["dv"], ["dv"])
        for cc in (1.0 / 11, 1.0 / 9, 1.0 / 7, 1.0 / 5, 1.0 / 3):
            self.stt(q_, q_, cc, z2_, ALU.add, ALU.mult, ["dv"], ["dv"])
        self.stt(q_, q_, 1.0, z_, ALU.add, ALU.mult, ["dv"], ["dv"])
        self.ts(self.dv[:, 16:32], q_, -8.0, None, ALU.mult, None, ["dv"], ["dv"])
        self.ts(self.dv[:, 0:16], q_, -16.0, None, ALU.mult, None, ["dv"], ["dv"])
        self.ts(self.dv[:, 32:48], self.vec[:, V_BA:V_BA + 16], 0.5, None, ALU.mult, None, ["cst", "dv"], ["dv"])
        self.ts(self.dv[:, 48:64], self.vec[:, V_BX:V_BX + 16], 0.5, None, ALU.mult, None, ["cst", "dv"], ["dv"])

    @staticmethod
    def zip_run(gens, weights=None):
        gens = list(gens)
        weights = list(weights) if weights else [1] * len(gens)
        alive = [True] * len(gens)
        while any(alive):
            for i, g in enumerate(gens):
                for _ in range(weights[i]):
                    if not alive[i]:
                        break
                    try:
                        next(g)
                    except StopIteration:
                        alive[i] = False

    def out_proj_ln(self, tts, nk, wsrc, rhs_fn, rhs_keys_fn, ln_col, tmp_off):
        P = self.P
        zb = [self.view(tmp_off + i * 1024, [128, 512], BF16) for i in range(3)]
        zq = [self.view(tmp_off + 3072 + i * 1024, [128, 512], BF16) for i in range(3)]
        o = tmp_off + 6144
        mean = [self.view(o + i * 2048, [128, 512], F32) for i in range(2)]
        var = [self.view(o + 4096 + i * 2048, [128, 512], F32) for i in range(2)]
        tmp = [self.view(o + 8192 + i * 2048, [128, 512], F32) for i in range(4)]
        zring, tring = self.zring, self.tring
        sbank = [(6, 7), (2, 3)]
        ybank = self.ybank

        def mloop():
            pending = []

            def flush():
                for (m, ti, zi) in pending:
                    b1, b2 = sbank[ti]
                    self.mm(self.ps[b1][:], self.onesb[:], zb[zi][:], m == 0, m == 7, [("zb", zi), "cb"], self.bk(b1), inc=True)
                    self.mm(self.ps[b2][:], self.onesb[:], zq[zi][:], m == 0, m == 7, [("zq", zi), "cb"], self.bk(b2), inc=True)
                pending.clear()

            for m in range(8):
                w, wkey = self.wload(wsrc(m), nk * 128)
                for ti, tt in enumerate(tts):
                    yb = 4 + ybank.next()
                    tsl = slice(tt * 512, (tt + 1) * 512)
                    for k in range(nk):
                        self.mm(self.ps[yb][:], w[:, k * 128:(k + 1) * 128], rhs_fn(k, tt), k == 0, k == nk - 1,
                                [wkey] + rhs_keys_fn(k, tt), [("ps", yb)])
                    flush()
                    hk = ("h32", m, tt)
                    self.stt(self.hT32[:, m, tsl], self.hT32[:, m, tsl], ALPHA, self.ps[yb][:], ALU.mult, ALU.add,
                             [hk, ("ps", yb)], [hk])
                    zi = zring.next()
                    self.act(zb[zi][:], self.hT32[:, m, tsl], AF.Copy, [hk], [("zb", zi)])
                    self.act(zq[zi][:], self.hT32[:, m, tsl], AF.Square, [hk], [("zq", zi)])
                    pending.append((m, ti, zi))
                    yield
            flush()

        def tail(ti, tt):
            b1, b2 = sbank[ti]
            tsl = slice(tt * 512, (tt + 1) * 512)
            mk, vk = ("lnmean", ti), ("lnvar", ti)
            self.act(mean[ti][:], self.ps[b1][:], AF.Identity, self.bk(b1), [mk], scale=1.0 / D)
            yield
            self.act(var[ti][:], self.ps[b2][:], AF.Identity, self.bk(b2), [vk], scale=1.0 / D, bias=EPS)
            yield
            ti_ = tring.next()
            self.tt(tmp[ti_][:], mean[ti][:], mean[ti][:], ALU.mult, [mk], [("lntmp", ti_)])
            yield
            self.tt(var[ti][:], var[ti][:], tmp[ti_][:], ALU.subtract, [vk, ("lntmp", ti_)], [vk])
            yield
            self.act(var[ti][:], var[ti][:], AF.Sqrt, [vk], [vk])
            yield
            P.op("dve", lambda e: e.reciprocal(var[ti][:], var[ti][:]), reads=[vk], writes=[vk])
            yield
            self.tt(mean[ti][:], mean[ti][:], var[ti][:], ALU.mult, [mk, vk], [mk])
            for m in range(8):
                yield
                hk = ("h32", m, tt)
                ti_ = tring.next()
                tk = ("lntmp", ti_)
                self.tt(tmp[ti_][:], self.hT32[:, m, tsl], var[ti][:], ALU.mult, [hk, vk], [tk])
                yield
                self.tt(tmp[ti_][:], tmp[ti_][:], mean[ti][:], ALU.subtract, [tk, mk], [tk])
                g = self.vcol(V_LNG + ln_col * 8 + m)
                b = self.vcol(V_LNB + ln_col * 8 + m)
                self.act(self.hT32[:, m, tsl], tmp[ti_][:], AF.Identity, [tk, "cst"], [hk], scale=g, bias=b)
                self.act(self.hTb[:, m, tsl], tmp[ti_][:], AF.Identity, [tk, "cst"], [("hb", m, tt)], scale=g, bias=b)

        def tails():
            gens = [tail(ti, tt) for ti, tt in enumerate(tts)]
            while gens:
                for g_ in list(gens):
                    try:
                        next(g_)
                        yield
                    except StopIteration:
                        gens.remove(g_)

        return mloop(), tails()

    def mixer_out(self, nk, wsrc, rhs_fn, rhs_keys_fn, ln_col, tmp_off):
        m0, t0 = self.out_proj_ln([0, 1], nk, wsrc, rhs_fn, rhs_keys_fn, ln_col, tmp_off)
        m1, t1 = self.out_proj_ln([2, 3], nk, wsrc, rhs_fn, rhs_keys_fn, ln_col, tmp_off)
        self.zip_run([m0])
        self.zip_run([t0, m1], [3, 1])
        self.P.snapshot_early()
        self.zip_run([t1])

    def ffn(self, li):
        P = self.P
        ACT_OFF = 0
        SG_OFF = 45056
        TMP_OFF = 49152
        actT = self.view(ACT_OFF, [128, NF, 1024], BF16)
        sg = [self.view(SG_OFF + i * 2048, [128, 512], F32) for i in range(2)]
        sgr = Ring(2)
        gb, ub = Ring(2), Ring(2)

        def inproj(tts):
            for f in range(NF):
                w, wkey = self.wload(self.fwin[li, f], 2048)
                for ti, tt in enumerate(tts):
                    g = gb.next()
                    u = 2 + ub.next()
                    tsl = slice(tt * 512, (tt + 1) * 512)
                    for k in range(8):
                        self.mm(self.ps[g][:], w[:, k * 128:(k + 1) * 128], self.hTb[:, k, tsl], k == 0, k == 7,
                                [wkey, ("hb", k, tt)], [("ps", g)])
                    for k in range(8):
                        self.mm(self.ps[u][:], w[:, 1024 + k * 128:1024 + (k + 1) * 128], self.hTb[:, k, tsl], k == 0, k == 7,
                                [wkey, ("hb", k, tt)], [("ps", u)])
                    si = sgr.next()
                    self.act(sg[si][:], self.ps[g][:], AF.Silu, [("ps", g)], [("sg", si)])
                    self.tt(actT[:, f, ti * 512:(ti + 1) * 512], sg[si][:], self.ps[u][:], ALU.mult,
                            [("sg", si), ("ps", u)], [("actT", f, ti)])
                    yield

        parts = []
        for grp in range(2):
            tts = [2 * grp, 2 * grp + 1]
            parts.append(self.out_proj_ln(tts, NF, lambda m: self.fwout[li, m],
                                          lambda k, tt: actT[:, k, (tt % 2) * 512:(tt % 2 + 1) * 512],
                                          lambda k, tt: [("actT", k, tt % 2)], li * 2 + 1, TMP_OFF))
        P.default_early = True
        self.zip_run([inproj([0, 1])])
        P.default_early = False
        self.zip_run([parts[0][0]])
        self.zip_run([parts[0][1], inproj([2, 3])], [2, 1])
        self.zip_run([parts[1][0]])
        P.snapshot_early()
        self.zip_run([parts[1][1]])

    def attn(self, li):
        P = self.P
        j = li // 2
        QT = [self.view(0 + i * 12288, [128, S], BF16) for i in range(2)]
        KT = [self.view(4096 + i * 12288, [128, S], BF16) for i in range(2)]
        VV = [self.view(8192 + i * 12288, [128, 16, 128], BF16) for i in range(2)]
        OT_OFF = 24576
        oT = self.view(OT_OFF, [128, 8, S], BF16)
        o = OT_OFF + 32768
        PT = [self.view(o + i * 512, [128, 256], BF16) for i in range(6)]
        o += 6 * 512
        lsT = self.view(o, [8, 2, S], BF16)
        o += 8192
        gm = self.view(o, [128, 128], F32); o += 512
        cmp_ = self.view(o, [128, 1024], F32); o += 4096
        rank = self.view(o, [128, 128], F32); o += 512
        lsel = [self.view(o + i * 512, [128, 128], F32) for i in range(2)]; o += 1024
        ksum = self.view(o, [128, 8], F32); o += 32
        kmT = [self.view(o + i * 32, [128, 8], BF16) for i in range(2)]; o += 64
        rden = [self.view(o + i * 1024, [128, 256], F32) for i in range(2)]; o += 2048
        acc = [[self.view(o + (2 * i + k) * 1024, [128, 256], F32) for k in range(2)] for i in range(2)]; o += 4096
        accb = [self.view(o + i * 512, [128, 256], BF16) for i in range(2)]; o += 1024
        assert o <= self.ARENA_BYTES
        TMP_OFF = 57344
        projb = Ring(2)
        ptr = Ring(6)
        sbk = Ring(3)
        sbanks = [2, 4, 5]
        rdr = Ring(2)
        past = self.cf[:, C_PAST:C_PAST + 128]

        def proj(h):
            hb = h % 2
            w, wkey = self.wload(self.wqkv[j, h], 3072)
            for tt in range(4):
                b = projb.next()
                tsl = slice(tt * 512, (tt + 1) * 512)
                for k in range(8):
                    self.mm(self.ps[b][:], w[:, k * 128:(k + 1) * 128], self.hTb[:, k, tsl], k == 0, k == 7,
                            [wkey, ("hb", k, tt)], [("ps", b)])
                self.act(QT[hb][:, tsl], self.ps[b][:], AF.Copy, [("ps", b)], [("QT", hb, tt)])
            for tt in range(4):
                b = projb.next()
                tsl = slice(tt * 512, (tt + 1) * 512)
                for k in range(8):
                    self.mm(self.ps[b][:], w[:, 1024 + k * 128:1024 + (k + 1) * 128], self.hTb[:, k, tsl], k == 0, k == 7,
                            [wkey, ("hb", k, tt)], [("ps", b)])
                P.op("dve", lambda e, b=b, tt=tt: e.tensor_reduce(
                    out=ksum[:, 2 * tt:2 * tt + 2], in_=self.ps[b][:].rearrange("p (a b) -> p a b", b=256),
                    axis=AX.X, op=ALU.add), reads=[], writes=[("ksum", tt), ("ps", b)])
                self.act(KT[hb][:, tsl], self.ps[b][:], AF.Copy, [], [("KT", hb, tt), ("ps", b)])
            self.ts(kmT[hb][:], ksum[:], 1.0 / 256, None, ALU.mult, None, [("ksum", t) for t in range(4)], [("kmT", hb)], ss=True)
            for g4 in range(4):
                b = projb.next()
                for t4 in range(4):
                    t16 = g4 * 4 + t4
                    for k in range(8):
                        self.mm(self.ps[b][:, t4 * 128:(t4 + 1) * 128], self.hTb[:, k, t16 * 128:(t16 + 1) * 128],
                                w[:, 2048 + k * 128:2048 + (k + 1) * 128], k == 0, k == 7,
                                [wkey, ("hb", k, t16 // 4)], [("ps", b)])
                P.op("dve", lambda e, b=b, g4=g4: e.tensor_copy(
                    VV[hb][:, g4 * 4:(g4 + 1) * 4, :].rearrange("p a b -> p (a b)"), self.ps[b][:]),
                    reads=[("ps", b)], writes=[("V", hb, g4)])

        def gate_a(h):
            hb = h % 2
            for t in range(16):
                self.mm(self.ps[3][:, t * 8:(t + 1) * 8], QT[hb][:, t * 128:(t + 1) * 128], kmT[hb][:], True, True,
                        [("QT", hb, t // 4), ("kmT", hb)], [("ps", 3)], inc=(t == 15))
            self.tt(gm[:], self.ps[3][:, 0:128], past, ALU.add, [("ps", 3), "cst"], ["gm"], ss=True)
            g3 = gm[:].rearrange("p (t n) -> p t n", n=8)
            in0 = g3.unsqueeze(2).broadcast_to([128, 16, 8, 8])
            in1 = g3.unsqueeze(3).broadcast_to([128, 16, 8, 8])
            self.tt(cmp_[:].rearrange("p (t n m) -> p t n m", n=8, m=8), in0, in1, ALU.is_gt, ["gm"], ["cmp"], ss=True)
            P.op("dve", lambda e: e.tensor_reduce(out=rank[:], in_=cmp_[:].rearrange("p (a m) -> p a m", m=8),
                                                  axis=AX.X, op=ALU.add), reads=["cmp"], writes=["rank"], ss=True)
            self.ts(lsel[hb][:], rank[:], 2.5, NEG, ALU.is_gt, ALU.mult, ["rank"], [("lsel", hb)], ss=True)

        def gate_b(h):
            hb = h % 2
            for g4 in range(4):
                for t4 in range(4):
                    t = g4 * 4 + t4
                    P.op("pe", lambda e, t=t, t4=t4: e.transpose(self.ps[3][0:8, t4 * 128:(t4 + 1) * 128],
                                                                 lsel[hb][:, t * 8:(t + 1) * 8], self.cf[:, C_ID:C_ID + 128]),
                         reads=[("lsel", hb), "cst"], writes=[("ps", 3)], inc=(t4 == 3))
                self.act(lsT[0:8, hb, g4 * 512:(g4 + 1) * 512], self.ps[3][0:8, :], AF.Copy, [("ps", 3)], [("lsT", hb, g4)])

        def attention(h):
            hb = h % 2
            deferred = []

            def finish(qb, ob, ai):
                qsl = slice(qb * 256, (qb + 1) * 256)
                self.tt(accb[ai][:], acc[ai][0][:], acc[ai][1][:], ALU.add, [("accA", ai), ("accB", ai)], [("accb", ai)])
                self.mm(self.ps[ob][:, 256:512], self.onesb[:], accb[ai][:], False, True,
                        [("accb", ai), "cb"], [("ps", ob)], inc=True, sgc=True)
                ri = rdr.next()
                P.op("dve", lambda e: e.reciprocal(rden[ri][:], self.ps[ob][:, 256:512]),
                     reads=[("ps", ob)], writes=[("rden", ri)])
                self.tt(oT[:, h, qsl], self.ps[ob][:, 0:256], rden[ri][:], ALU.mult,
                        [("ps", ob), ("rden", ri)], [("oT", h, qb // 2)], ss=True)

            for qb in range(8):
                ob = 6 + qb % 2
                ai = qb % 2
                qsl = slice(qb * 256, (qb + 1) * 256)
                nkb = qb + 1
                pend = None

                def pv(kb, pslots, first, last):
                    for jj in range(2):
                        jt = 2 * kb + jj
                        st, sp_ = first and jj == 0, last and jj == 1
                        self.mm(self.ps[ob][:, 0:256], VV[hb][:, jt, :], PT[pslots[jj]][:], st, sp_,
                                [("V", hb, jt // 4), ("PT", pslots[jj])], [("ps", ob)], inc=True, sgc=True)
                        eng, key = ("dve", ("accA", ai)) if jj == 0 else ("pool", ("accB", ai))
                        dst = acc[ai][jj]
                        src = PT[pslots[jj]]
                        if kb == 0:
                            P.op(eng, lambda e, dst=dst, src=src: e.tensor_copy(dst[:], src[:]),
                                 reads=[("PT", pslots[jj])], writes=[key])
                        else:
                            self.tt(dst[:], dst[:], src[:], ALU.add, [("PT", pslots[jj]), key], [key], eng=eng)

                for kb in range(nkb):
                    sb_ = sbanks[sbk.next()]
                    slots = []
                    for jj in range(2):
                        jt = 2 * kb + jj
                        csl = slice(jj * 256, (jj + 1) * 256)
                        nomask = kb < qb and qb <= 3
                        self.mm(self.ps[sb_][:, csl], KT[hb][:, jt * 128:(jt + 1) * 128], QT[hb][:, qsl], True, nomask,
                                [("KT", hb, jt // 4), ("QT", hb, qb // 2)], [("ps", sb_)], inc=nomask)
                        if nomask:
                            pass
                        elif kb < qb:
                            self.mm(self.ps[sb_][:, csl], self.e8b[0:8, kb, :], lsT[0:8, hb, qsl], False, True,
                                    ["cb", ("lsT", hb, qb // 2)], [("ps", sb_)], inc=True)
                        else:
                            self.mm(self.ps[sb_][:, csl], self.identb[:], self.causb[:, jj, :], False, True,
                                    ["cb"], [("ps", sb_)], inc=True)
                    for jj in range(2):
                        jt = 2 * kb + jj
                        csl = slice(jj * 256, (jj + 1) * 256)
                        pi = ptr.next()
                        slots.append(pi)
                        r = jt - 2 * qb - 1 + 16
                        bias = self.cf[:, C_BIAS + h * 17 + r:C_BIAS + h * 17 + r + 1]
                        self.act(PT[pi][:], self.ps[sb_][:, csl], AF.Exp, [("ps", sb_), "cst"], [("PT", pi)],
                                 bias=bias, scale=SCALE)
                    if kb == 0 and deferred:
                        finish(*deferred.pop())
                    if pend is not None:
                        pv(pend[0], pend[1], pend[0] == 0, False)
                    pend = (kb, slots)
                pv(pend[0], pend[1], pend[0] == 0, True)
                deferred.append((qb, ob, ai))
            finish(*deferred.pop())

        P.default_early = True
        proj(0)
        P.default_early = False
        gate_a(0)
        gate_b(0)
        for h in range(NH):
            if h + 1 < NH:
                proj(h + 1)
                gate_a(h + 1)
            attention(h)
            if h + 1 < NH:
                gate_b(h + 1)
        self.mixer_out(8, lambda m: self.wo[j, m], lambda k, tt: oT[:, k, tt * 512:(tt + 1) * 512],
                       lambda k, tt: [("oT", k, tt)], li * 2, TMP_OFF)

    def lru(self, li):
        P = self.P
        j = li // 2
        HS = 1024
        mT = self.view(0, [128, 8, S], BF16)
        o = 32768
        A2 = [self.view(o + i * 2048, [128, 512], F32) for i in range(2)]; o += 4096
        OM2 = [self.view(o + i * 2048, [128, 512], F32) for i in range(2)]; o += 4096
        IX2 = [self.view(o + i * 2048, [128, 512], F32) for i in range(2)]; o += 4096
        GT2 = [self.view(o + i * 2048, [128, 512], F32) for i in range(2)]; o += 4096
        xpad = [self.view(o + i * 2064, [128, 516], F32) for i in range(3)]; o += 6192
        xc = [self.view(o + i * 2048, [128, 512], F32) for i in range(4)]; o += 8192
        xcb = [self.view(o + i * 1024, [128, 512], BF16) for i in range(2)]; o += 2048
        t1 = [self.view(o + i * 2048, [128, 512], F32) for i in range(6)]; o += 12288
        U = [self.view(o + i * 2048, [128, 512], F32) for i in range(2)]; o += 4096
        wab = self.view(o, [128, 8, 128], BF16); o += 2048
        wxb = self.view(o, [128, 8, 128], BF16); o += 2048
        carry = self.view(o, [128, 2], F32); o += 8
        assert o <= self.ARENA_BYTES, o
        TMP_OFF = 57344
        P.op("pool", lambda e: e.dma_start(out=wab[:].rearrange("p a b -> p (a b)"), in_=self.lwa[j], max_dma_last_dim=4096),
             writes=["wab"], dma="dga")
        P.op("pool", lambda e: e.dma_start(out=wxb[:].rearrange("p a b -> p (a b)"), in_=self.lwx[j], max_dma_last_dim=4096),
             writes=["wxb"], dma="dgb")
        xbk, ybk = Ring(2), Ring(2)
        xpr, xcr, xcbr, t1r, ur = Ring(3), Ring(4), Ring(2), Ring(6), Ring(2)
        allk = lambda n: [(n, t) for t in range(2)]
        st = {}
        cur = {"w": None, "prev_xp": None}

        def consts(c):
            return dict(cw=[self.vcol(V_CW + (j * 4 + tap) * 8 + c) for tap in range(4)],
                        cb=self.vcol(V_CB + j * 8 + c),
                        hcl=self.dv[:, 16 + j * 8 + c:16 + j * 8 + c + 1],
                        hba=self.dv[:, 32 + j * 8 + c:32 + j * 8 + c + 1],
                        hbx=self.dv[:, 48 + j * 8 + c:48 + j * 8 + c + 1])

        def s1(n, c, tt, xi, pxi):
            w, wkey = cur["w"]
            k_ = consts(c)
            tsl = slice(tt * 512, (tt + 1) * 512)
            xb_ = xbk.next()
            yb_ = 2 + ybk.next()
            ci = xcr.next()
            bi = xcbr.next()
            rb, ib = 4 + 2 * (n % 2), 5 + 2 * (n % 2)
            st[n] = dict(yb=yb_, ci=ci, rb=rb, ib=ib)
            for k in range(8):
                self.mm(self.ps[xb_][:], w[:, k * 128:(k + 1) * 128], self.hTb[:, k, tsl], k == 0, k == 7,
                        [wkey, ("hb", k, tt)], [("ps", xb_)])
            for k in range(8):
                self.mm(self.ps[yb_][:], w[:, 1024 + k * 128:1024 + (k + 1) * 128], self.hTb[:, k, tsl], k == 0, k == 7,
                        [wkey, ("hb", k, tt)], [("ps", yb_)])
            yield
            xk = ("xpad", xi)
            self.act(xpad[xi][:, 3:515], self.ps[xb_][:], AF.Copy, [("ps", xb_)], [xk])
            ck = ("xc", ci)
            self.act(xc[ci][:], self.ps[xb_][:], AF.Identity, [("ps", xb_), "cst"], [ck], scale=k_["cw"][3], bias=k_["cb"])
            yield
            if tt == 0:
                P.op("dve", lambda e: e.memset(xpad[xi][:, 0:3], 0.0), writes=[xk])
            else:
                P.op("dve", lambda e: e.tensor_copy(xpad[xi][:, 0:3], xpad[pxi][:, 512:515]),
                     reads=[("xpad", pxi)], writes=[xk])
            for tap in (2, 1, 0):
                yield
                self.stt(xc[ci][:], xpad[xi][:, tap:tap + 512], k_["cw"][tap], xc[ci][:], ALU.mult, ALU.add,
                         [xk, ck, "cst"], [ck])
            yield
            self.act(xcb[bi][:], xc[ci][:], AF.Copy, [ck], [("xcb", bi)])
            yield
            self.mm(self.ps[rb][:], wab[:, c, :], xcb[bi][:], True, True, ["wab", ("xcb", bi)], [("ps", rb)])
            self.mm(self.ps[ib][:], wxb[:, c, :], xcb[bi][:], True, True, ["wxb", ("xcb", bi)], [("ps", ib)])

        def s2(n, c, tt):
            k_ = consts(c)
            d = st.pop(n)
            yb_, ci, rb, ib = d["yb"], d["ci"], d["rb"], d["ib"]
            ck = ("xc", ci)
            tl = n % 2
            lsl = slice(0, 512)
            A_, OM, IX, GT = A2[tl], OM2[tl], IX2[tl], GT2[tl]
            ta, tb, tg, ua = t1r.next(), t1r.next(), t1r.next(), ur.next()
            tk, tbk, tgk, uk = ("t1", ta), ("t1", tb), ("t1", tg), ("U", ua)
            self.act(t1[ta][:], self.ps[rb][:], AF.Tanh, [("ps", rb), "dv"], [tk], scale=0.5, bias=k_["hba"])
            yield
            self.act(U[ua][:], t1[ta][:], AF.Identity, [tk, "dv"], [uk], scale=k_["hcl"], bias=k_["hcl"])
            yield
            self.act(t1[ta][:], U[ua][:], AF.Identity, [uk], [tk], scale=1.0 / 24.0, bias=1.0 / 6.0)
            yield
            self.act(t1[tb][:], self.ps[ib][:], AF.Tanh, [("ps", ib), "dv"], [tbk], scale=0.5, bias=k_["hbx"])
            yield
            self.act(GT[:, lsl], self.ps[yb_][:], AF.Gelu_apprx_tanh, [("ps", yb_)], [("GT", tl)])
            for cc in (None, 0.5, 1.0):
                yield
                if cc is None:
                    self.tt(t1[ta][:], t1[ta][:], U[ua][:], ALU.mult, [tk, uk], [tk])
                else:
                    self.stt(t1[ta][:], t1[ta][:], cc, U[ua][:], ALU.add, ALU.mult, [tk, uk], [tk])
            yield
            self.stt(U[ua][:], t1[ta][:], 2.0, t1[ta][:], ALU.add, ALU.mult, [tk], [uk])
            yield
            self.stt(IX[:, lsl], t1[tb][:], 1.0, xc[ci][:], ALU.add, ALU.mult, [tbk, ck], [("IX", tl)])
            yield
            self.ts(A_[:, lsl], t1[ta][:], 1.0, None, ALU.add, None, [tk], [("A", tl)])
            yield
            self.ts(OM[:, lsl], U[ua][:], -1.0, 0.0, ALU.mult, ALU.max, [uk], [("OM", tl)])

        def s3(n, c, tt):
            tl = n % 2
            A_, OM, IX, GT = A2[tl], OM2[tl], IX2[tl], GT2[tl]
            tsl = slice(tt * 512, (tt + 1) * 512)
            self.act(OM[:], OM[:], AF.Sqrt, [("OM", tl)], [("OM", tl)])
            yield
            yield
            self.stt(IX[:], OM[:], 0.5, IX[:], ALU.mult, ALU.mult, [("OM", tl), ("IX", tl)], [("IX", tl)])
            yield
            yield
            init = 0.0 if tt == 0 else carry[:, 0:1]
            P.op("dve", lambda e: e.tensor_tensor_scan(out=OM[:], data0=A_[:], data1=IX[:], initial=init,
                                                       op0=ALU.mult, op1=ALU.add),
                 reads=[("A", tl), ("IX", tl), "carry"], writes=[("OM", tl)])
            if tt < 3:
                P.op("pool", lambda e: e.tensor_copy(carry[:, 0:1], OM[:, 511:512]), reads=[("OM", tl)], writes=["carry"])
            self.tt(mT[:, c, tsl], OM[:], GT[:], ALU.mult, [("OM", tl), ("GT", tl)], [("mT", c, tt)], eng="pool")

        def zip_run(gens):
            gens = list(gens)
            while gens:
                for g in list(gens):
                    try:
                        next(g)
                    except StopIteration:
                        gens.remove(g)

        tiles = [(c, tt) for c in range(8) for tt in range(4)]
        NT_ = len(tiles)
        for n in range(NT_ + 2):
            gens = []
            if n < NT_:
                c, tt = tiles[n]
                if tt == 0:
                    cur["w"] = self.wload(self.lwin[j, c], 2048)
                xi = xpr.next()
                pxi = cur["prev_xp"]
                cur["prev_xp"] = xi
                gens.append(s1(n, c, tt, xi, pxi))
            if 1 <= n <= NT_:
                gens.append(s2(n - 1, *tiles[n - 1]))
            if n >= 2:
                gens.append(s3(n - 2, *tiles[n - 2]))
            zip_run(gens)
        if self.dbg == "lru_mT":
            for c in range(8):
                tok = P.op("pool", lambda e, c=c: e.dma_start(out=self.dbgT[c * 128:(c + 1) * 128, :], in_=mT[:, c, :], max_dma_last_dim=4096),
                           reads=[("mT", c, t) for t in range(4)], dma="ddbg")
            P.final_waits("pool", [tok])
            return
        self.mixer_out(8, lambda m: self.lwout[j, m], lambda k, tt: mT[:, k, tt * 512:(tt + 1) * 512],
                       lambda k, tt: [("mT", k, tt)], li * 2, TMP_OFF)


def _consts():
    cf = np.zeros((128, NCF), np.float32)
    cf[:, C_ID:C_ID + 128] = np.eye(128, dtype=np.float32)
    p = np.arange(128, dtype=np.float64)
    for h in range(NH):
        slope = 2.0 ** (-8.0 * (h + 1) / NH)
        for r in range(17):
            cf[:, C_BIAS + h * 17 + r] = slope * (p + 128.0 * (r - 16))
    past = np.zeros((16, 8), np.float32)
    for t in range(16):
        for n in range(8):
            if n >= t // 2:
                past[t, n] = -1e30
    cf[:, C_PAST:C_PAST + 128] = past.reshape(1, 128)
    q = np.arange(256)
    for half in range(2):
        k = half * 128 + np.arange(128)
        cf[:, C_CAUS + half * 256:C_CAUS + (half + 1) * 256] = np.where(q[None, :] >= k[:, None], 0.0, NEG)
    e8 = np.zeros((8, 8, 128), np.float32)
    for kb in range(8):
        e8[kb, kb, :] = 1.0
    return cf, e8.reshape(8, 1024)


def _prep_weights(inp):
    f = lambda a: np.ascontiguousarray(a, dtype=np.float32)
    out = {}
    w = inp["attn_w_qkv"].reshape(2, 8, 128, 3, NH, 128)
    out["wqkv"] = f(w.transpose(0, 4, 2, 3, 1, 5).reshape(2, NH, 128, 3072))
    w = inp["attn_w_o"].reshape(2, 8, 128, 8, 128)
    out["wo"] = f(w.transpose(0, 3, 2, 1, 4).reshape(2, 8, 128, 1024))
    w = inp["lru_w_in"].reshape(2, 8, 128, 2, 8, 128)
    out["lwin"] = f(w.transpose(0, 4, 2, 3, 1, 5).reshape(2, 8, 128, 2048))
    w = inp["lru_w_out"].reshape(2, 8, 128, 8, 128)
    out["lwout"] = f(w.transpose(0, 3, 2, 1, 4).reshape(2, 8, 128, 1024))
    out["lwa"] = f(inp["lru_w_a"].transpose(0, 2, 1, 3).reshape(2, 128, 1024))
    out["lwx"] = f(inp["lru_w_x"].transpose(0, 2, 1, 3).reshape(2, 128, 1024))
    w = inp["ffn_w_in"].reshape(DEPTH, 8, 128, 2, NF, 128)
    out["fwin"] = f(w.transpose(0, 4, 2, 3, 1, 5).reshape(DEPTH, NF, 128, 2048))
    w = inp["ffn_w_out"].reshape(DEPTH, NF, 128, 8, 128)
    out["fwout"] = f(w.transpose(0, 3, 2, 1, 4).reshape(DEPTH, 8, 128, FF))
    vecs = np.zeros((128, NV), np.float32)
    pc = lambda a: a.reshape(a.shape[:-1] + (8, 128))
    vecs[:, V_LNG:V_LNG + 64] = pc(inp["ln_g"]).transpose(3, 0, 1, 2).reshape(128, 64)
    vecs[:, V_LNB:V_LNB + 64] = pc(inp["ln_b"]).transpose(3, 0, 1, 2).reshape(128, 64)
    vecs[:, V_CW:V_CW + 64] = pc(inp["lru_conv_w"]).transpose(3, 0, 1, 2).reshape(128, 64)
    vecs[:, V_CB:V_CB + 16] = pc(inp["lru_conv_b"]).transpose(2, 0, 1).reshape(128, 16)
    vecs[:, V_BA:V_BA + 16] = pc(inp["lru_b_a"]).transpose(2, 0, 1).reshape(128, 16)
    vecs[:, V_BX:V_BX + 16] = pc(inp["lru_b_x"]).transpose(2, 0, 1).reshape(128, 16)
    vecs[:, V_LAM:V_LAM + 16] = pc(inp["lru_lambda"]).transpose(2, 0, 1).reshape(128, 16)
    out["vecs"] = vecs
    out["cf"], out["e8"] = _consts()
    return out


ALL_PHASES = [("attn", 0), ("ffn", 0), ("lru", 1), ("ffn", 1), ("attn", 2), ("ffn", 2), ("lru", 3), ("ffn", 3)]
_NC_CACHE = {}


def run(inputs, phases=None, trace=False, dbg=None):
    phases = ALL_PHASES if phases is None else phases
    key = (tuple(phases), dbg)
    if key not in _NC_CACHE:
        _NC_CACHE[key] = K(phases, dbg).build()
    nc = _NC_CACHE[key]
    inp = {k: np.asarray(v) for k, v in inputs.items()}
    shared = _prep_weights(inp)
    x = np.asarray(inp["x"], dtype=np.float32)
    in_maps = []
    for b in range(8):
        m = dict(shared)
        m["xT"] = np.ascontiguousarray(x[b].T)
        in_maps.append(m)
    res = run_bass_kernel_spmd(nc, in_maps, core_ids=list(range(8)), trace=trace)
    out = np.stack([np.ascontiguousarray(r["outT"].T) for r in res.results], axis=0).astype(np.float32)
    if dbg:
        out = np.stack([np.ascontiguousarray(r["dbgT"].T) for r in res.results], axis=0).astype(np.float32)
    return out, res


def kernel(**inputs):
    out, _ = run(inputs)
    return out
```

```python
import numpy as np
from contextlib import ExitStack
import concourse.bass as bass
import concourse.mybir as mybir
from concourse.bass_utils import run_bass_kernel_spmd

F32 = mybir.dt.float32
BF16 = mybir.dt.bfloat16
AF = mybir.ActivationFunctionType
ALU = mybir.AluOpType
AX = mybir.AxisListType

D = 1024
S = 2048
DEPTH = 4
NH = 8
FF = 2816
NF = FF // 128
ALPHA = float((2 * DEPTH) ** 0.25)
EPS = 1e-5
SCALE = float(128 ** -0.5)
NEG = -30000.0
ENGS = ["pe", "act", "dve", "pool", "sp"]

V_LNG = 0
V_LNB = 64
V_CW = 128
V_CB = 192
V_BA = 208
V_BX = 224
V_LAM = 240
NV = 256
C_ID = 0
C_BIAS = 128
C_PAST = 264
C_CAUS = 392
NCF = 904


class Prog:
    def __init__(self, same_engine_sync=False):
        self.ops = {e: [] for e in ENGS}
        self.cnt = {e: 0 for e in ENGS}
        self.dcnt = {}
        self.res = {}
        self.waited = {e: {} for e in ENGS}
        self.same_engine_sync = same_engine_sync
        self.barrier = {}
        self.early = {}
        self.early_next = None
        self.default_early = False

    def _counts(self):
        return {"e_" + e: self.cnt[e] for e in ENGS if self.cnt[e] > 0}

    def snapshot_early(self):
        self.early_next = self._counts()

    def phase_barrier(self):
        self.barrier = self._counts()
        self.early = self.early_next if self.early_next is not None else self.barrier
        self.early_next = None

    def _st(self, key):
        st = self.res.get(key)
        if st is None:
            st = {"w": None, "r": {}}
            self.res[key] = st
        return st

    def op(self, eng, fn, reads=(), writes=(), dma=None, inc=True, nobarrier=False, ss=False):
        need = {} if nobarrier else dict(self.early if self.default_early else self.barrier)

        def add(tok):
            if tok is None:
                return
            s, v = tok
            if need.get(s, 0) < v:
                need[s] = v

        for r in reads:
            add(self._st(r)["w"])
        for w in writes:
            st = self._st(w)
            add(st["w"])
            for s, v in st["r"].items():
                add((s, v))
        if dma is None:
            if inc:
                self.cnt[eng] += 1
                tok = ("e_" + eng, self.cnt[eng])
                inc = 1
            else:
                tok = ("e_" + eng, self.cnt[eng] + 1)
                inc = 0
        else:
            self.dcnt[dma] = self.dcnt.get(dma, 0) + 16
            tok = (dma, self.dcnt[dma])
            inc = 16
        waits = []
        wd = self.waited[eng]
        for s, v in need.items():
            if s == "e_" + eng and (eng == "pe" or not (self.same_engine_sync or ss)):
                continue
            if wd.get(s, 0) >= v:
                continue
            wd[s] = v
            waits.append((s, v))
        self.ops[eng].append((fn, waits, tok[0], inc))
        for r in reads:
            st = self._st(r)
            if st["r"].get(tok[0], 0) < tok[1]:
                st["r"][tok[0]] = tok[1]
        for w in writes:
            st = self._st(w)
            st["w"] = tok
            st["r"] = {}
        return tok

    def final_waits(self, eng, toks):
        self.ops[eng].append((None, list(toks), None, 0))

    def emit(self, nc, es):
        sems = {}
        names = ["e_" + e for e in ENGS if self.cnt[e] > 0] + list(self.dcnt.keys())
        for n in names:
            sems[n] = es.enter_context(nc.semaphore(n))
        block = es.enter_context(nc.Block())
        engmap = {"pe": block.tensor, "act": block.scalar, "dve": block.vector,
                  "pool": block.gpsimd, "sp": block.sync}
        for e in ENGS:
            ops = self.ops[e]
            if not ops:
                continue

            def body(eng, ops=ops):
                for fn, waits, sname, inc in ops:
                    for s, v in waits:
                        eng.wait_ge(sems[s], v)
                    if fn is not None:
                        ins = fn(eng)
                        if inc:
                            ins.then_inc(sems[sname], inc)

            engmap[e](body)


class Ring:
    def __init__(self, n):
        self.n = n
        self.i = -1

    def next(self):
        self.i = (self.i + 1) % self.n
        return self.i


class K:
    NSLOT = 3
    SLOT_ELEMS = 3072
    ARENA_BYTES = 85 * 1024

    def __init__(self, phases, dbg=None):
        self.phases = phases
        self.dbg = dbg
        self.nc = nc = bass.Bass("TRN2", target_bir_lowering=False)
        self.P = Prog()
        dt = lambda name, shape, kind="ExternalInput": nc.dram_tensor(name, shape, F32, kind=kind).ap()
        self.xT = dt("xT", [D, S])
        self.outT = dt("outT", [D, S], "ExternalOutput")
        self.wqkv = dt("wqkv", [2, NH, 128, 3072])
        self.wo = dt("wo", [2, 8, 128, 1024])
        self.lwin = dt("lwin", [2, 8, 128, 2048])
        self.lwout = dt("lwout", [2, 8, 128, 1024])
        self.lwa = dt("lwa", [2, 128, 1024])
        self.lwx = dt("lwx", [2, 128, 1024])
        self.fwin = dt("fwin", [DEPTH, NF, 128, 2048])
        self.fwout = dt("fwout", [DEPTH, 8, 128, FF])
        self.vecs_d = dt("vecs", [128, NV])
        self.cf_d = dt("cf", [128, NCF])
        self.e8_d = dt("e8", [8, 1024])
        if dbg:
            self.dbgT = dt("dbgT", [D, S], "ExternalOutput")

    def view(self, off, shape, dtype):
        nel = int(np.prod(shape[1:]))
        nbytes = nel * (4 if dtype == F32 else 2)
        assert off % 4 == 0 and off + nbytes <= self.ARENA_BYTES, (off, nbytes)
        a = self.arena[0:shape[0], off // 4:(off + nbytes + 3) // 4]
        if dtype != F32:
            a = a.bitcast(dtype)
        if len(shape) == 3:
            a = a.rearrange("p (a b) -> p a b", b=shape[2])
        elif len(shape) == 4:
            a = a.rearrange("p (a b c) -> p a b c", b=shape[2], c=shape[3])
        return a

    def wload(self, src, nel):
        s = self.wring_i.next()
        dst = self.wring[:, s, 0:nel]
        key = ("w", s)
        self.P.op("pool", lambda e: e.dma_start(out=dst, in_=src, max_dma_last_dim=4096),
                  writes=[key], dma="dw%d" % s, nobarrier=True)
        return dst, key

    def mm(self, out, lhsT, rhs, start, stop, reads, writes, inc=None, sgc=False):
        if inc is None:
            inc = stop
        self.P.op("pe", lambda e: e.matmul(out, lhsT=lhsT, rhs=rhs, start=start, stop=stop, skip_group_check=sgc),
                  reads=reads, writes=writes, inc=inc)

    def act(self, out, in_, func, reads, writes, bias=None, scale=None, ss=False):
        kw = {}
        if bias is not None:
            kw["bias"] = bias
        if scale is not None:
            kw["scale"] = scale
        self.P.op("act", lambda e: e.activation(out=out, in_=in_, func=func, **kw), reads=reads, writes=writes, ss=ss)

    def tt(self, out, in0, in1, op, reads, writes, eng="dve", ss=False):
        self.P.op(eng, lambda e: e.tensor_tensor(out=out, in0=in0, in1=in1, op=op), reads=reads, writes=writes, ss=ss)

    def ts(self, out, in0, s1, s2, op0, op1, reads, writes, eng="dve", ss=False):
        if op1 is None:
            self.P.op(eng, lambda e: e.tensor_scalar(out=out, in0=in0, scalar1=s1, scalar2=None, op0=op0),
                      reads=reads, writes=writes, ss=ss)
        else:
            self.P.op(eng, lambda e: e.tensor_scalar(out=out, in0=in0, scalar1=s1, scalar2=s2, op0=op0, op1=op1),
                      reads=reads, writes=writes, ss=ss)

    def stt(self, out, in0, scalar, in1, op0, op1, reads, writes, ss=False):
        self.P.op("dve", lambda e: e.scalar_tensor_tensor(out=out, in0=in0, scalar=scalar, in1=in1, op0=op0, op1=op1),
                  reads=reads, writes=writes, ss=ss)

    @staticmethod
    def bk(b):
        return [("ps", b)]

    def vcol(self, col):
        return self.vec[:, col:col + 1]

    def build(self):
        nc, P = self.nc, self.P
        with ExitStack() as es:
            sb = lambda name, shape, dtype: es.enter_context(nc.sbuf_tensor(name, shape, dtype))
            self.hT32 = sb("hT32", [128, 8, S], F32)
            self.hTb = sb("hTb", [128, 8, S], BF16)
            self.vec = sb("vec_sb", [128, NV], F32)
            self.dv = sb("dv", [128, 64], F32)
            self.dv2 = sb("dv2", [128, 64], F32)
            self.cf = sb("cf_sb", [128, NCF], F32)
            self.identb = sb("identb", [128, 128], BF16)
            self.onesb = sb("onesb", [128, 128], BF16)
            self.causb = sb("causb", [128, 2, 256], BF16)
            self.e8b = sb("e8b", [8, 8, 128], BF16)
            self.cpow = sb("cpow", [128, 2], F32)
            self.wring = sb("wring", [128, self.NSLOT, self.SLOT_ELEMS], BF16)
            self.wring_i = Ring(self.NSLOT)
            self.zring, self.tring, self.ybank = Ring(3), Ring(4), Ring(2)
            self.arena = sb("arena", [128, self.ARENA_BYTES // 4], F32)
            self.ps = [es.enter_context(nc.psum_tensor("ps%d" % i, [128, 512], F32)) for i in range(8)]

            P.same_engine_sync = True
            self.setup()
            for ph in self.phases:
                kind, li = ph
                P.phase_barrier()
                if kind == "attn":
                    self.attn(li)
                elif kind == "lru":
                    self.lru(li)
                elif kind == "ffn":
                    self.ffn(li)
            ov = self.outT.rearrange("(c p) t -> p c t", p=128)
            toks = []
            for t in range(4):
                toks.append(P.op("sp", lambda e, t=t: e.dma_start(out=ov[:, :, t * 512:(t + 1) * 512], in_=self.hT32[:, :, t * 512:(t + 1) * 512]),
                                 reads=[("h32", c, t) for c in range(8)], dma="dout%d" % t))
            P.final_waits("sp", toks)
            P.emit(nc, es)
        return nc

    def setup(self):
        P = self.P
        h32keys = [("h32", c, t) for c in range(8) for t in range(4)]
        hbkeys = [("hb", c, t) for c in range(8) for t in range(4)]
        P.op("sp", lambda e: e.dma_start(out=self.vec[:], in_=self.vecs_d), writes=["cst"], dma="dc")
        P.op("sp", lambda e: e.dma_start(out=self.cf[:], in_=self.cf_d), writes=["cst"], dma="dc")
        P.op("pool", lambda e: e.dma_start(out=self.e8b[:].rearrange("p a b -> p (a b)"), in_=self.e8_d), writes=["cb"], dma="dg0")
        xv = self.xT.rearrange("(c p) t -> p c t", p=128)
        for t in range(4):
            P.op("sp", lambda e, t=t: e.dma_start(out=self.hT32[:, :, t * 512:(t + 1) * 512], in_=xv[:, :, t * 512:(t + 1) * 512]),
                 writes=[("h32", c, t) for c in range(8)], dma="dx%d" % t)
        for t in range(4):
            for c in range(8):
                tsl = slice(t * 512, (t + 1) * 512)
                if c % 2 == 0:
                    P.op("dve", lambda e, c=c, tsl=tsl: e.tensor_copy(self.hTb[:, c, tsl], self.hT32[:, c, tsl]),
                         reads=[("h32", c, t)], writes=[("hb", c, t)])
                else:
                    P.op("act", lambda e, c=c, tsl=tsl: e.copy(self.hTb[:, c, tsl], self.hT32[:, c, tsl]),
                         reads=[("h32", c, t)], writes=[("hb", c, t)])
        P.op("dve", lambda e: e.tensor_copy(self.identb[:], self.cf[:, C_ID:C_ID + 128]), reads=["cst"], writes=["cb"])
        P.op("dve", lambda e: e.tensor_copy(self.causb[:].rearrange("p a b -> p (a b)"), self.cf[:, C_CAUS:C_CAUS + 512]),
             reads=["cst"], writes=["cb"])
        P.op("dve", lambda e: e.memset(self.onesb[:], 1.0), writes=["cb"])
        P.op("dve", lambda e: e.memset(self.cpow[:, 0:1], -0.5), writes=["cb"])
        P.op("dve", lambda e: e.memset(self.cpow[:, 1:2], 0.5), writes=["cb"])
        lam = self.vec[:, V_LAM:V_LAM + 16]
        e_, z_, z2_, q_ = (self.dv2[:, i * 16:(i + 1) * 16] for i in range(4))
        self.act(e_, lam, AF.Exp, ["cst"], ["dv"], scale=-1.0)
        self.ts(z_, e_, 2.0, None, ALU.add, None, ["dv"], ["dv"])
        P.op("dve", lambda e: e.reciprocal(z_, z_), reads=["dv"], writes=["dv"])
        self.tt(z_, z_, e_, ALU.mult, ["dv"], ["dv"])
        self.tt(z2_, z_, z_, ALU.mult, ["dv"], ["dv"])
        self.ts(q_, z2_, 1.0 / 13.0, None, ALU.mult, None, ["dv"], ["dv"])
        for cc in (1.0 / 11, 1.0 / 9, 1.0 / 7, 1.0 / 5, 1.0 / 3):
            self.stt(q_, q_, cc, z2_, ALU.add, ALU.mult, ["dv"], ["dv"])
        self.stt(q_, q_, 1.0, z_, ALU.add, ALU.mult, ["dv"], ["dv"])
        self.ts(self.dv[:, 16:32], q_, -8.0, None, ALU.mult, None, ["dv"], ["dv"])
        self.ts(self.dv[:, 0:16], q_, -16.0, None, ALU.mult, None, ["dv"], ["dv"])
        self.ts(self.dv[:, 32:48], self.vec[:, V_BA:V_BA + 16], 0.5, None, ALU.mult, None, ["cst", "dv"], ["dv"])
        self.ts(self.dv[:, 48:64], self.vec[:, V_BX:V_BX + 16], 0.5, None, ALU.mult, None, ["cst", "dv"], ["dv"])

    @staticmethod
    def zip_run(gens, weights=None):
        gens = list(gens)
        weights = list(weights) if weights else [1] * len(gens)
        alive = [True] * len(gens)
        while any(alive):
            for i, g in enumerate(gens):
                for _ in range(weights[i]):
                    if not alive[i]:
                        break
                    try:
                        next(g)
                    except StopIteration:
                        alive[i] = False

    def out_proj_ln(self, tts, nk, wsrc, rhs_fn, rhs_keys_fn, ln_col, tmp_off):
        P = self.P
        zb = [self.view(tmp_off + i * 1024, [128, 512], BF16) for i in range(3)]
        zq = [self.view(tmp_off + 3072 + i * 1024, [128, 512], BF16) for i in range(3)]
        o = tmp_off + 6144
        mean = [self.view(o + i * 2048, [128, 512], F32) for i in range(2)]
        var = [self.view(o + 4096 + i * 2048, [128, 512], F32) for i in range(2)]
        tmp = [self.view(o + 8192 + i * 2048, [128, 512], F32) for i in range(4)]
        zring, tring = self.zring, self.tring
        sbank = [(6, 7), (2, 3)]
        ybank = self.ybank

        def mloop():
            pending = []

            def flush():
                for (m, ti, zi) in pending:
                    b1, b2 = sbank[ti]
                    self.mm(self.ps[b1][:], self.onesb[:], zb[zi][:], m == 0, m == 7, [("zb", zi), "cb"], self.bk(b1), inc=True)
                    self.mm(self.ps[b2][:], self.onesb[:], zq[zi][:], m == 0, m == 7, [("zq", zi), "cb"], self.bk(b2), inc=True)
                pending.clear()

            for m in range(8):
                w, wkey = self.wload(wsrc(m), nk * 128)
                for ti, tt in enumerate(tts):
                    yb = 4 + ybank.next()
                    tsl = slice(tt * 512, (tt + 1) * 512)
                    for k in range(nk):
                        self.mm(self.ps[yb][:], w[:, k * 128:(k + 1) * 128], rhs_fn(k, tt), k == 0, k == nk - 1,
                                [wkey] + rhs_keys_fn(k, tt), [("ps", yb)])
                    flush()
                    hk = ("h32", m, tt)
                    self.stt(self.hT32[:, m, tsl], self.hT32[:, m, tsl], ALPHA, self.ps[yb][:], ALU.mult, ALU.add,
                             [hk, ("ps", yb)], [hk])
                    zi = zring.next()
                    self.act(zb[zi][:], self.hT32[:, m, tsl], AF.Copy, [hk], [("zb", zi)])
                    self.act(zq[zi][:], self.hT32[:, m, tsl], AF.Square, [hk], [("zq", zi)])
                    pending.append((m, ti, zi))
                    yield
            flush()

        def tail(ti, tt):
            b1, b2 = sbank[ti]
            tsl = slice(tt * 512, (tt + 1) * 512)
            mk, vk = ("lnmean", ti), ("lnvar", ti)
            self.act(mean[ti][:], self.ps[b1][:], AF.Identity, self.bk(b1), [mk], scale=1.0 / D)
            yield
            self.act(var[ti][:], self.ps[b2][:], AF.Identity, self.bk(b2), [vk], scale=1.0 / D, bias=EPS)
            yield
            ti_ = tring.next()
            self.tt(tmp[ti_][:], mean[ti][:], mean[ti][:], ALU.mult, [mk], [("lntmp", ti_)])
            yield
            self.tt(var[ti][:], var[ti][:], tmp[ti_][:], ALU.subtract, [vk, ("lntmp", ti_)], [vk])
            yield
            self.act(var[ti][:], var[ti][:], AF.Sqrt, [vk], [vk])
            yield
            P.op("dve", lambda e: e.reciprocal(var[ti][:], var[ti][:]), reads=[vk], writes=[vk])
            yield
            self.tt(mean[ti][:], mean[ti][:], var[ti][:], ALU.mult, [mk, vk], [mk])
            for m in range(8):
                yield
                hk = ("h32", m, tt)
                ti_ = tring.next()
                tk = ("lntmp", ti_)
                self.tt(tmp[ti_][:], self.hT32[:, m, tsl], var[ti][:], ALU.mult, [hk, vk], [tk])
                yield
                self.tt(tmp[ti_][:], tmp[ti_][:], mean[ti][:], ALU.subtract, [tk, mk], [tk])
                g = self.vcol(V_LNG + ln_col * 8 + m)
                b = self.vcol(V_LNB + ln_col * 8 + m)
                self.act(self.hT32[:, m, tsl], tmp[ti_][:], AF.Identity, [tk, "cst"], [hk], scale=g, bias=b)
                self.act(self.hTb[:, m, tsl], tmp[ti_][:], AF.Identity, [tk, "cst"], [("hb", m, tt)], scale=g, bias=b)

        def tails():
            gens = [tail(ti, tt) for ti, tt in enumerate(tts)]
            while gens:
                for g_ in list(gens):
                    try:
                        next(g_)
                        yield
                    except StopIteration:
                        gens.remove(g_)

        return mloop(), tails()

    def mixer_out(self, nk, wsrc, rhs_fn, rhs_keys_fn, ln_col, tmp_off):
        m0, t0 = self.out_proj_ln([0, 1], nk, wsrc, rhs_fn, rhs_keys_fn, ln_col, tmp_off)
        m1, t1 = self.out_proj_ln([2, 3], nk, wsrc, rhs_fn, rhs_keys_fn, ln_col, tmp_off)
        self.zip_run([m0])
        self.zip_run([t0, m1], [3, 1])
        self.P.snapshot_early()
        self.zip_run([t1])

    def ffn(self, li):
        P = self.P
        ACT_OFF = 0
        SG_OFF = 45056
        TMP_OFF = 49152
        actT = self.view(ACT_OFF, [128, NF, 1024], BF16)
        sg = [self.view(SG_OFF + i * 2048, [128, 512], F32) for i in range(2)]
        sgr = Ring(2)
        gb, ub = Ring(2), Ring(2)

        def inproj(tts):
            for f in range(NF):
                w, wkey = self.wload(self.fwin[li, f], 2048)
                for ti, tt in enumerate(tts):
                    g = gb.next()
                    u = 2 + ub.next()
                    tsl = slice(tt * 512, (tt + 1) * 512)
                    for k in range(8):
                        self.mm(self.ps[g][:], w[:, k * 128:(k + 1) * 128], self.hTb[:, k, tsl], k == 0, k == 7,
                                [wkey, ("hb", k, tt)], [("ps", g)])
                    for k in range(8):
                        self.mm(self.ps[u][:], w[:, 1024 + k * 128:1024 + (k + 1) * 128], self.hTb[:, k, tsl], k == 0, k == 7,
                                [wkey, ("hb", k, tt)], [("ps", u)])
                    si = sgr.next()
                    self.act(sg[si][:], self.ps[g][:], AF.Silu, [("ps", g)], [("sg", si)])
                    self.tt(actT[:, f, ti * 512:(ti + 1) * 512], sg[si][:], self.ps[u][:], ALU.mult,
                            [("sg", si), ("ps", u)], [("actT", f, ti)])
                    yield

        parts = []
        for grp in range(2):
            tts = [2 * grp, 2 * grp + 1]
            parts.append(self.out_proj_ln(tts, NF, lambda m: self.fwout[li, m],
                                          lambda k, tt: actT[:, k, (tt % 2) * 512:(tt % 2 + 1) * 512],
                                          lambda k, tt: [("actT", k, tt % 2)], li * 2 + 1, TMP_OFF))
        P.default_early = True
        self.zip_run([inproj([0, 1])])
        P.default_early = False
        self.zip_run([parts[0][0]])
        self.zip_run([parts[0][1], inproj([2, 3])], [2, 1])
        self.zip_run([parts[1][0]])
        P.snapshot_early()
        self.zip_run([parts[1][1]])

    def attn(self, li):
        P = self.P
        j = li // 2
        QT = [self.view(0 + i * 12288, [128, S], BF16) for i in range(2)]
        KT = [self.view(4096 + i * 12288, [128, S], BF16) for i in range(2)]
        VV = [self.view(8192 + i * 12288, [128, 16, 128], BF16) for i in range(2)]
        OT_OFF = 24576
        oT = self.view(OT_OFF, [128, 8, S], BF16)
        o = OT_OFF + 32768
        PT = [self.view(o + i * 512, [128, 256], BF16) for i in range(6)]
        o += 6 * 512
        lsT = self.view(o, [8, 2, S], BF16)
        o += 8192
        gm = self.view(o, [128, 128], F32); o += 512
        cmp_ = self.view(o, [128, 1024], F32); o += 4096
        rank = self.view(o, [128, 128], F32); o += 512
        lsel = [self.view(o + i * 512, [128, 128], F32) for i in range(2)]; o += 1024
        ksum = self.view(o, [128, 8], F32); o += 32
        kmT = [self.view(o + i * 32, [128, 8], BF16) for i in range(2)]; o += 64
        rden = [self.view(o + i * 1024, [128, 256], F32) for i in range(2)]; o += 2048
        assert o <= self.ARENA_BYTES
        TMP_OFF = 57344
        projb = Ring(2)
        ptr = Ring(6)
        sbk = Ring(3)
        sbanks = [2, 4, 5]
        rdr = Ring(2)
        past = self.cf[:, C_PAST:C_PAST + 128]

        def proj(h):
            hb = h % 2
            w, wkey = self.wload(self.wqkv[j, h], 3072)
            for tt in range(4):
                b = projb.next()
                tsl = slice(tt * 512, (tt + 1) * 512)
                for k in range(8):
                    self.mm(self.ps[b][:], w[:, k * 128:(k + 1) * 128], self.hTb[:, k, tsl], k == 0, k == 7,
                            [wkey, ("hb", k, tt)], [("ps", b)])
                self.act(QT[hb][:, tsl], self.ps[b][:], AF.Copy, [("ps", b)], [("QT", hb, tt)])
            for tt in range(4):
                b = projb.next()
                tsl = slice(tt * 512, (tt + 1) * 512)
                for k in range(8):
                    self.mm(self.ps[b][:], w[:, 1024 + k * 128:1024 + (k + 1) * 128], self.hTb[:, k, tsl], k == 0, k == 7,
                            [wkey, ("hb", k, tt)], [("ps", b)])
                P.op("dve", lambda e, b=b, tt=tt: e.tensor_reduce(
                    out=ksum[:, 2 * tt:2 * tt + 2], in_=self.ps[b][:].rearrange("p (a b) -> p a b", b=256),
                    axis=AX.X, op=ALU.add), reads=[], writes=[("ksum", tt), ("ps", b)])
                self.act(KT[hb][:, tsl], self.ps[b][:], AF.Copy, [], [("KT", hb, tt), ("ps", b)])
            self.ts(kmT[hb][:], ksum[:], 1.0 / 256, None, ALU.mult, None, [("ksum", t) for t in range(4)], [("kmT", hb)], ss=True)
            for g4 in range(4):
                b = projb.next()
                for t4 in range(4):
                    t16 = g4 * 4 + t4
                    for k in range(8):
                        self.mm(self.ps[b][:, t4 * 128:(t4 + 1) * 128], self.hTb[:, k, t16 * 128:(t16 + 1) * 128],
                                w[:, 2048 + k * 128:2048 + (k + 1) * 128], k == 0, k == 7,
                                [wkey, ("hb", k, t16 // 4)], [("ps", b)])
                P.op("dve", lambda e, b=b, g4=g4: e.tensor_copy(
                    VV[hb][:, g4 * 4:(g4 + 1) * 4, :].rearrange("p a b -> p (a b)"), self.ps[b][:]),
                    reads=[("ps", b)], writes=[("V", hb, g4)])

        def gate_a(h):
            hb = h % 2
            for t in range(16):
                self.mm(self.ps[3][:, t * 8:(t + 1) * 8], QT[hb][:, t * 128:(t + 1) * 128], kmT[hb][:], True, True,
                        [("QT", hb, t // 4), ("kmT", hb)], [("ps", 3)], inc=(t == 15))
            self.tt(gm[:], self.ps[3][:, 0:128], past, ALU.add, [("ps", 3), "cst"], ["gm"], ss=True)
            g3 = gm[:].rearrange("p (t n) -> p t n", n=8)
            in0 = g3.unsqueeze(2).broadcast_to([128, 16, 8, 8])
            in1 = g3.unsqueeze(3).broadcast_to([128, 16, 8, 8])
            self.tt(cmp_[:].rearrange("p (t n m) -> p t n m", n=8, m=8), in0, in1, ALU.is_gt, ["gm"], ["cmp"], ss=True)
            P.op("dve", lambda e: e.tensor_reduce(out=rank[:], in_=cmp_[:].rearrange("p (a m) -> p a m", m=8),
                                                  axis=AX.X, op=ALU.add), reads=["cmp"], writes=["rank"], ss=True)
            self.ts(lsel[hb][:], rank[:], 2.5, NEG, ALU.is_gt, ALU.mult, ["rank"], [("lsel", hb)], ss=True)

        def gate_b(h):
            hb = h % 2
            for g4 in range(4):
                for t4 in range(4):
                    t = g4 * 4 + t4
                    P.op("pe", lambda e, t=t, t4=t4: e.transpose(self.ps[3][0:8, t4 * 128:(t4 + 1) * 128],
                                                                 lsel[hb][:, t * 8:(t + 1) * 8], self.cf[:, C_ID:C_ID + 128]),
                         reads=[("lsel", hb), "cst"], writes=[("ps", 3)], inc=(t4 == 3))
                self.act(lsT[0:8, hb, g4 * 512:(g4 + 1) * 512], self.ps[3][0:8, :], AF.Copy, [("ps", 3)], [("lsT", hb, g4)])

        def attention(h):
            hb = h % 2
            for qb in range(8):
                ob = 6 + qb % 2
                qsl = slice(qb * 256, (qb + 1) * 256)
                nkb = qb + 1
                pend = None

                def pv(kb, pslots, first, last):
                    for jj in range(2):
                        jt = 2 * kb + jj
                        st, sp_ = first and jj == 0, last and jj == 1
                        self.mm(self.ps[ob][:, 0:256], VV[hb][:, jt, :], PT[pslots[jj]][:], st, sp_,
                                [("V", hb, jt // 4), ("PT", pslots[jj])], [("ps", ob)], inc=True, sgc=True)
                        self.mm(self.ps[ob][:, 256:512], self.onesb[:], PT[pslots[jj]][:], False, sp_,
                                [("PT", pslots[jj]), "cb"], [("ps", ob)], inc=True, sgc=True)

                for kb in range(nkb):
                    sb_ = sbanks[sbk.next()]
                    slots = []
                    for jj in range(2):
                        jt = 2 * kb + jj
                        csl = slice(jj * 256, (jj + 1) * 256)
                        nomask = kb < qb and qb <= 3
                        self.mm(self.ps[sb_][:, csl], KT[hb][:, jt * 128:(jt + 1) * 128], QT[hb][:, qsl], True, nomask,
                                [("KT", hb, jt // 4), ("QT", hb, qb // 2)], [("ps", sb_)], inc=nomask)
                        if nomask:
                            pass
                        elif kb < qb:
                            self.mm(self.ps[sb_][:, csl], self.e8b[0:8, kb, :], lsT[0:8, hb, qsl], False, True,
                                    ["cb", ("lsT", hb, qb // 2)], [("ps", sb_)], inc=True)
                        else:
                            self.mm(self.ps[sb_][:, csl], self.identb[:], self.causb[:, jj, :], False, True,
                                    ["cb"], [("ps", sb_)], inc=True)
                    for jj in range(2):
                        jt = 2 * kb + jj
                        csl = slice(jj * 256, (jj + 1) * 256)
                        pi = ptr.next()
                        slots.append(pi)
                        r = jt - 2 * qb - 1 + 16
                        bias = self.cf[:, C_BIAS + h * 17 + r:C_BIAS + h * 17 + r + 1]
                        self.act(PT[pi][:], self.ps[sb_][:, csl], AF.Exp, [("ps", sb_), "cst"], [("PT", pi)],
                                 bias=bias, scale=SCALE)
                    if pend is not None:
                        pv(pend[0], pend[1], pend[0] == 0, False)
                    pend = (kb, slots)
                pv(pend[0], pend[1], pend[0] == 0, True)
                ri = rdr.next()
                P.op("dve", lambda e, ri=ri, ob=ob: e.reciprocal(rden[ri][:], self.ps[ob][:, 256:512]),
                     reads=[("ps", ob)], writes=[("rden", ri)])
                self.tt(oT[:, h, qsl], self.ps[ob][:, 0:256], rden[ri][:], ALU.mult,
                        [("ps", ob), ("rden", ri)], [("oT", h, qb // 2)], ss=True)

        P.default_early = True
        proj(0)
        P.default_early = False
        gate_a(0)
        gate_b(0)
        for h in range(NH):
            if h + 1 < NH:
                proj(h + 1)
                gate_a(h + 1)
            attention(h)
            if h + 1 < NH:
                gate_b(h + 1)
        self.mixer_out(8, lambda m: self.wo[j, m], lambda k, tt: oT[:, k, tt * 512:(tt + 1) * 512],
                       lambda k, tt: [("oT", k, tt)], li * 2, TMP_OFF)

    def lru(self, li):
        P = self.P
        j = li // 2
        HS = 1024
        mT = self.view(0, [128, 8, S], BF16)
        o = 32768
        A2 = [self.view(o + i * 2048, [128, 512], F32) for i in range(2)]; o += 4096
        OM2 = [self.view(o + i * 2048, [128, 512], F32) for i in range(2)]; o += 4096
        IX2 = [self.view(o + i * 2048, [128, 512], F32) for i in range(2)]; o += 4096
        GT2 = [self.view(o + i * 2048, [128, 512], F32) for i in range(2)]; o += 4096
        xpad = [self.view(o + i * 2064, [128, 516], F32) for i in range(3)]; o += 6192
        xc = [self.view(o + i * 2048, [128, 512], F32) for i in range(4)]; o += 8192
        xcb = [self.view(o + i * 1024, [128, 512], BF16) for i in range(2)]; o += 2048
        t1 = [self.view(o + i * 2048, [128, 512], F32) for i in range(6)]; o += 12288
        U = [self.view(o + i * 2048, [128, 512], F32) for i in range(2)]; o += 4096
        wab = self.view(o, [128, 8, 128], BF16); o += 2048
        wxb = self.view(o, [128, 8, 128], BF16); o += 2048
        carry = self.view(o, [128, 2], F32); o += 8
        assert o <= self.ARENA_BYTES, o
        TMP_OFF = 57344
        P.op("pool", lambda e: e.dma_start(out=wab[:].rearrange("p a b -> p (a b)"), in_=self.lwa[j], max_dma_last_dim=4096),
             writes=["wab"], dma="dga")
        P.op("pool", lambda e: e.dma_start(out=wxb[:].rearrange("p a b -> p (a b)"), in_=self.lwx[j], max_dma_last_dim=4096),
             writes=["wxb"], dma="dgb")
        xbk, ybk = Ring(2), Ring(2)
        xpr, xcr, xcbr, t1r, ur = Ring(3), Ring(4), Ring(2), Ring(6), Ring(2)
        allk = lambda n: [(n, t) for t in range(2)]
        st = {}
        cur = {"w": None, "prev_xp": None}

        def consts(c):
            return dict(cw=[self.vcol(V_CW + (j * 4 + tap) * 8 + c) for tap in range(4)],
                        cb=self.vcol(V_CB + j * 8 + c),
                        hcl=self.dv[:, 16 + j * 8 + c:16 + j * 8 + c + 1],
                        hba=self.dv[:, 32 + j * 8 + c:32 + j * 8 + c + 1],
                        hbx=self.dv[:, 48 + j * 8 + c:48 + j * 8 + c + 1])

        def s1(n, c, tt, xi, pxi):
            w, wkey = cur["w"]
            k_ = consts(c)
            tsl = slice(tt * 512, (tt + 1) * 512)
            xb_ = xbk.next()
            yb_ = 2 + ybk.next()
            ci = xcr.next()
            bi = xcbr.next()
            rb, ib = 4 + 2 * (n % 2), 5 + 2 * (n % 2)
            st[n] = dict(yb=yb_, ci=ci, rb=rb, ib=ib)
            for k in range(8):
                self.mm(self.ps[xb_][:], w[:, k * 128:(k + 1) * 128], self.hTb[:, k, tsl], k == 0, k == 7,
                        [wkey, ("hb", k, tt)], [("ps", xb_)])
            for k in range(8):
                self.mm(self.ps[yb_][:], w[:, 1024 + k * 128:1024 + (k + 1) * 128], self.hTb[:, k, tsl], k == 0, k == 7,
                        [wkey, ("hb", k, tt)], [("ps", yb_)])
            yield
            xk = ("xpad", xi)
            self.act(xpad[xi][:, 3:515], self.ps[xb_][:], AF.Copy, [("ps", xb_)], [xk])
            ck = ("xc", ci)
            self.act(xc[ci][:], self.ps[xb_][:], AF.Identity, [("ps", xb_), "cst"], [ck], scale=k_["cw"][3], bias=k_["cb"])
            yield
            if tt == 0:
                P.op("dve", lambda e: e.memset(xpad[xi][:, 0:3], 0.0), writes=[xk])
            else:
                P.op("dve", lambda e: e.tensor_copy(xpad[xi][:, 0:3], xpad[pxi][:, 512:515]),
                     reads=[("xpad", pxi)], writes=[xk])
            for tap in (2, 1, 0):
                yield
                self.stt(xc[ci][:], xpad[xi][:, tap:tap + 512], k_["cw"][tap], xc[ci][:], ALU.mult, ALU.add,
                         [xk, ck, "cst"], [ck])
            yield
            self.act(xcb[bi][:], xc[ci][:], AF.Copy, [ck], [("xcb", bi)])
            yield
            self.mm(self.ps[rb][:], wab[:, c, :], xcb[bi][:], True, True, ["wab", ("xcb", bi)], [("ps", rb)])
            self.mm(self.ps[ib][:], wxb[:, c, :], xcb[bi][:], True, True, ["wxb", ("xcb", bi)], [("ps", ib)])

        def s2(n, c, tt):
            k_ = consts(c)
            d = st.pop(n)
            yb_, ci, rb, ib = d["yb"], d["ci"], d["rb"], d["ib"]
            ck = ("xc", ci)
            tl = n % 2
            lsl = slice(0, 512)
            A_, OM, IX, GT = A2[tl], OM2[tl], IX2[tl], GT2[tl]
            ta, tb, tg, ua = t1r.next(), t1r.next(), t1r.next(), ur.next()
            tk, tbk, tgk, uk = ("t1", ta), ("t1", tb), ("t1", tg), ("U", ua)
            self.act(t1[ta][:], self.ps[rb][:], AF.Tanh, [("ps", rb), "dv"], [tk], scale=0.5, bias=k_["hba"])
            yield
            self.act(U[ua][:], t1[ta][:], AF.Identity, [tk, "dv"], [uk], scale=k_["hcl"], bias=k_["hcl"])
            yield
            self.act(t1[ta][:], U[ua][:], AF.Identity, [uk], [tk], scale=1.0 / 24.0, bias=1.0 / 6.0)
            yield
            self.act(t1[tb][:], self.ps[ib][:], AF.Tanh, [("ps", ib), "dv"], [tbk], scale=0.5, bias=k_["hbx"])
            yield
            self.act(GT[:, lsl], self.ps[yb_][:], AF.Gelu_apprx_tanh, [("ps", yb_)], [("GT", tl)])
            for cc in (None, 0.5, 1.0):
                yield
                if cc is None:
                    self.tt(t1[ta][:], t1[ta][:], U[ua][:], ALU.mult, [tk, uk], [tk])
                else:
                    self.stt(t1[ta][:], t1[ta][:], cc, U[ua][:], ALU.add, ALU.mult, [tk, uk], [tk])
            yield
            self.stt(U[ua][:], t1[ta][:], 2.0, t1[ta][:], ALU.add, ALU.mult, [tk], [uk])
            yield
            self.stt(IX[:, lsl], t1[tb][:], 1.0, xc[ci][:], ALU.add, ALU.mult, [tbk, ck], [("IX", tl)])
            yield
            self.ts(A_[:, lsl], t1[ta][:], 1.0, None, ALU.add, None, [tk], [("A", tl)])
            yield
            self.ts(OM[:, lsl], U[ua][:], -1.0, 0.0, ALU.mult, ALU.max, [uk], [("OM", tl)])

        def s3(n, c, tt):
            tl = n % 2
            A_, OM, IX, GT = A2[tl], OM2[tl], IX2[tl], GT2[tl]
            tsl = slice(tt * 512, (tt + 1) * 512)
            self.act(OM[:], OM[:], AF.Sqrt, [("OM", tl)], [("OM", tl)])
            yield
            yield
            self.stt(IX[:], OM[:], 0.5, IX[:], ALU.mult, ALU.mult, [("OM", tl), ("IX", tl)], [("IX", tl)])
            yield
            yield
            init = 0.0 if tt == 0 else carry[:, 0:1]
            P.op("dve", lambda e: e.tensor_tensor_scan(out=OM[:], data0=A_[:], data1=IX[:], initial=init,
                                                       op0=ALU.mult, op1=ALU.add),
                 reads=[("A", tl), ("IX", tl), "carry"], writes=[("OM", tl)])
            if tt < 3:
                P.op("pool", lambda e: e.tensor_copy(carry[:, 0:1], OM[:, 511:512]), reads=[("OM", tl)], writes=["carry"])
            self.tt(mT[:, c, tsl], OM[:], GT[:], ALU.mult, [("OM", tl), ("GT", tl)], [("mT", c, tt)], eng="pool")

        def zip_run(gens):
            gens = list(gens)
            while gens:
                for g in list(gens):
                    try:
                        next(g)
                    except StopIteration:
                        gens.remove(g)

        tiles = [(c, tt) for c in range(8) for tt in range(4)]
        NT_ = len(tiles)
        for n in range(NT_ + 2):
            gens = []
            if n < NT_:
                c, tt = tiles[n]
                if tt == 0:
                    cur["w"] = self.wload(self.lwin[j, c], 2048)
                xi = xpr.next()
                pxi = cur["prev_xp"]
                cur["prev_xp"] = xi
                gens.append(s1(n, c, tt, xi, pxi))
            if 1 <= n <= NT_:
                gens.append(s2(n - 1, *tiles[n - 1]))
            if n >= 2:
                gens.append(s3(n - 2, *tiles[n - 2]))
            zip_run(gens)
        if self.dbg == "lru_mT":
            for c in range(8):
                tok = P.op("pool", lambda e, c=c: e.dma_start(out=self.dbgT[c * 128:(c + 1) * 128, :], in_=mT[:, c, :], max_dma_last_dim=4096),
                           reads=[("mT", c, t) for t in range(4)], dma="ddbg")
            P.final_waits("pool", [tok])
            return
        self.mixer_out(8, lambda m: self.lwout[j, m], lambda k, tt: mT[:, k, tt * 512:(tt + 1) * 512],
                       lambda k, tt: [("mT", k, tt)], li * 2, TMP_OFF)


def _consts():
    cf = np.zeros((128, NCF), np.float32)
    cf[:, C_ID:C_ID + 128] = np.eye(128, dtype=np.float32)
    p = np.arange(128, dtype=np.float64)
    for h in range(NH):
        slope = 2.0 ** (-8.0 * (h + 1) / NH)
        for r in range(17):
            cf[:, C_BIAS + h * 17 + r] = slope * (p + 128.0 * (r - 16))
    past = np.zeros((16, 8), np.float32)
    for t in range(16):
        for n in range(8):
            if n >= t // 2:
                past[t, n] = -1e30
    cf[:, C_PAST:C_PAST + 128] = past.reshape(1, 128)
    q = np.arange(256)
    for half in range(2):
        k = half * 128 + np.arange(128)
        cf[:, C_CAUS + half * 256:C_CAUS + (half + 1) * 256] = np.where(q[None, :] >= k[:, None], 0.0, NEG)
    e8 = np.zeros((8, 8, 128), np.float32)
    for kb in range(8):
        e8[kb, kb, :] = 1.0
    return cf, e8.reshape(8, 1024)


def _prep_weights(inp):
    f = lambda a: np.ascontiguousarray(a, dtype=np.float32)
    out = {}
    w = inp["attn_w_qkv"].reshape(2, 8, 128, 3, NH, 128)
    out["wqkv"] = f(w.transpose(0, 4, 2, 3, 1, 5).reshape(2, NH, 128, 3072))
    w = inp["attn_w_o"].reshape(2, 8, 128, 8, 128)
    out["wo"] = f(w.transpose(0, 3, 2, 1, 4).reshape(2, 8, 128, 1024))
    w = inp["lru_w_in"].reshape(2, 8, 128, 2, 8, 128)
    out["lwin"] = f(w.transpose(0, 4, 2, 3, 1, 5).reshape(2, 8, 128, 2048))
    w = inp["lru_w_out"].reshape(2, 8, 128, 8, 128)
    out["lwout"] = f(w.transpose(0, 3, 2, 1, 4).reshape(2, 8, 128, 1024))
    out["lwa"] = f(inp["lru_w_a"].transpose(0, 2, 1, 3).reshape(2, 128, 1024))
    out["lwx"] = f(inp["lru_w_x"].transpose(0, 2, 1, 3).reshape(2, 128, 1024))
    w = inp["ffn_w_in"].reshape(DEPTH, 8, 128, 2, NF, 128)
    out["fwin"] = f(w.transpose(0, 4, 2, 3, 1, 5).reshape(DEPTH, NF, 128, 2048))
    w = inp["ffn_w_out"].reshape(DEPTH, NF, 128, 8, 128)
    out["fwout"] = f(w.transpose(0, 3, 2, 1, 4).reshape(DEPTH, 8, 128, FF))
    vecs = np.zeros((128, NV), np.float32)
    pc = lambda a: a.reshape(a.shape[:-1] + (8, 128))
    vecs[:, V_LNG:V_LNG + 64] = pc(inp["ln_g"]).transpose(3, 0, 1, 2).reshape(128, 64)
    vecs[:, V_LNB:V_LNB + 64] = pc(inp["ln_b"]).transpose(3, 0, 1, 2).reshape(128, 64)
    vecs[:, V_CW:V_CW + 64] = pc(inp["lru_conv_w"]).transpose(3, 0, 1, 2).reshape(128, 64)
    vecs[:, V_CB:V_CB + 16] = pc(inp["lru_conv_b"]).transpose(2, 0, 1).reshape(128, 16)
    vecs[:, V_BA:V_BA + 16] = pc(inp["lru_b_a"]).transpose(2, 0, 1).reshape(128, 16)
    vecs[:, V_BX:V_BX + 16] = pc(inp["lru_b_x"]).transpose(2, 0, 1).reshape(128, 16)
    vecs[:, V_LAM:V_LAM + 16] = pc(inp["lru_lambda"]).transpose(2, 0, 1).reshape(128, 16)
    out["vecs"] = vecs
    out["cf"], out["e8"] = _consts()
    return out


ALL_PHASES = [("attn", 0), ("ffn", 0), ("lru", 1), ("ffn", 1), ("attn", 2), ("ffn", 2), ("lru", 3), ("ffn", 3)]
_NC_CACHE = {}


def run(inputs, phases=None, trace=False, dbg=None):
    phases = ALL_PHASES if phases is None else phases
    key = (tuple(phases), dbg)
    if key not in _NC_CACHE:
        _NC_CACHE[key] = K(phases, dbg).build()
    nc = _NC_CACHE[key]
    inp = {k: np.asarray(v) for k, v in inputs.items()}
    shared = _prep_weights(inp)
    x = np.asarray(inp["x"], dtype=np.float32)
    in_maps = []
    for b in range(8):
        m = dict(shared)
        m["xT"] = np.ascontiguousarray(x[b].T)
        in_maps.append(m)
    res = run_bass_kernel_spmd(nc, in_maps, core_ids=list(range(8)), trace=trace)
    out = np.stack([np.ascontiguousarray(r["outT"].T) for r in res.results], axis=0).astype(np.float32)
    if dbg:
        out = np.stack([np.ascontiguousarray(r["dbgT"].T) for r in res.results], axis=0).astype(np.float32)
    return out, res


def kernel(**inputs):
    out, _ = run(inputs)
    return out
```

```python
import numpy as np
from contextlib import ExitStack
import concourse.bass as bass
import concourse.mybir as mybir
from concourse.bass_utils import run_bass_kernel_spmd

F32 = mybir.dt.float32
BF16 = mybir.dt.bfloat16
AF = mybir.ActivationFunctionType
ALU = mybir.AluOpType
AX = mybir.AxisListType

D = 1024
S = 2048
DEPTH = 4
NH = 8
FF = 2816
NF = FF // 128
ALPHA = float((2 * DEPTH) ** 0.25)
EPS = 1e-5
SCALE = float(128 ** -0.5)
NEG = -30000.0
ENGS = ["pe", "act", "dve", "pool", "sp"]

V_LNG = 0
V_LNB = 64
V_CW = 128
V_CB = 192
V_BA = 208
V_BX = 224
V_LAM = 240
NV = 256
C_ID = 0
C_BIAS = 128
C_PAST = 264
C_CAUS = 392
NCF = 904


class Prog:
    def __init__(self, same_engine_sync=False):
        self.ops = {e: [] for e in ENGS}
        self.cnt = {e: 0 for e in ENGS}
        self.dcnt = {}
        self.res = {}
        self.waited = {e: {} for e in ENGS}
        self.same_engine_sync = same_engine_sync
        self.barrier = {}
        self.early = {}
        self.early_next = None
        self.default_early = False

    def _counts(self):
        return {"e_" + e: self.cnt[e] for e in ENGS if self.cnt[e] > 0}

    def snapshot_early(self):
        self.early_next = self._counts()

    def phase_barrier(self):
        self.barrier = self._counts()
        self.early = self.early_next if self.early_next is not None else self.barrier
        self.early_next = None

    def _st(self, key):
        st = self.res.get(key)
        if st is None:
            st = {"w": None, "r": {}}
            self.res[key] = st
        return st

    def op(self, eng, fn, reads=(), writes=(), dma=None, inc=True, nobarrier=False, ss=False):
        need = {} if nobarrier else dict(self.early if self.default_early else self.barrier)

        def add(tok):
            if tok is None:
                return
            s, v = tok
            if need.get(s, 0) < v:
                need[s] = v

        for r in reads:
            add(self._st(r)["w"])
        for w in writes:
            st = self._st(w)
            add(st["w"])
            for s, v in st["r"].items():
                add((s, v))
        if dma is None:
            if inc:
                self.cnt[eng] += 1
                tok = ("e_" + eng, self.cnt[eng])
                inc = 1
            else:
                tok = ("e_" + eng, self.cnt[eng] + 1)
                inc = 0
        else:
            self.dcnt[dma] = self.dcnt.get(dma, 0) + 16
            tok = (dma, self.dcnt[dma])
            inc = 16
        waits = []
        wd = self.waited[eng]
        for s, v in need.items():
            if s == "e_" + eng and (eng == "pe" or not (self.same_engine_sync or ss)):
                continue
            if wd.get(s, 0) >= v:
                continue
            wd[s] = v
            waits.append((s, v))
        self.ops[eng].append((fn, waits, tok[0], inc))
        for r in reads:
            st = self._st(r)
            if st["r"].get(tok[0], 0) < tok[1]:
                st["r"][tok[0]] = tok[1]
        for w in writes:
            st = self._st(w)
            st["w"] = tok
            st["r"] = {}
        return tok

    def final_waits(self, eng, toks):
        self.ops[eng].append((None, list(toks), None, 0))

    def emit(self, nc, es):
        sems = {}
        names = ["e_" + e for e in ENGS if self.cnt[e] > 0] + list(self.dcnt.keys())
        for n in names:
            sems[n] = es.enter_context(nc.semaphore(n))
        block = es.enter_context(nc.Block())
        engmap = {"pe": block.tensor, "act": block.scalar, "dve": block.vector,
                  "pool": block.gpsimd, "sp": block.sync}
        for e in ENGS:
            ops = self.ops[e]
            if not ops:
                continue

            def body(eng, ops=ops):
                for fn, waits, sname, inc in ops:
                    for s, v in waits:
                        eng.wait_ge(sems[s], v)
                    if fn is not None:
                        ins = fn(eng)
                        if inc:
                            ins.then_inc(sems[sname], inc)

            engmap[e](body)


class Ring:
    def __init__(self, n):
        self.n = n
        self.i = -1

    def next(self):
        self.i = (self.i + 1) % self.n
        return self.i


class K:
    NSLOT = 3
    SLOT_ELEMS = 3072
    ARENA_BYTES = 85 * 1024

    def __init__(self, phases, dbg=None):
        self.phases = phases
        self.dbg = dbg
        self.nc = nc = bass.Bass("TRN2", target_bir_lowering=False)
        self.P = Prog()
        dt = lambda name, shape, kind="ExternalInput": nc.dram_tensor(name, shape, F32, kind=kind).ap()
        self.xT = dt("xT", [D, S])
        self.outT = dt("outT", [D, S], "ExternalOutput")
        self.wqkv = dt("wqkv", [2, NH, 128, 3072])
        self.wo = dt("wo", [2, 8, 128, 1024])
        self.lwin = dt("lwin", [2, 8, 128, 2048])
        self.lwout = dt("lwout", [2, 8, 128, 1024])
        self.lwa = dt("lwa", [2, 128, 1024])
        self.lwx = dt("lwx", [2, 128, 1024])
        self.fwin = dt("fwin", [DEPTH, NF, 128, 2048])
        self.fwout = dt("fwout", [DEPTH, 8, 128, FF])
        self.vecs_d = dt("vecs", [128, NV])
        self.cf_d = dt("cf", [128, NCF])
        self.e8_d = dt("e8", [8, 1024])
        if dbg:
            self.dbgT = dt("dbgT", [D, S], "ExternalOutput")

    def view(self, off, shape, dtype):
        nel = int(np.prod(shape[1:]))
        nbytes = nel * (4 if dtype == F32 else 2)
        assert off % 4 == 0 and off + nbytes <= self.ARENA_BYTES, (off, nbytes)
        a = self.arena[0:shape[0], off // 4:(off + nbytes + 3) // 4]
        if dtype != F32:
            a = a.bitcast(dtype)
        if len(shape) == 3:
            a = a.rearrange("p (a b) -> p a b", b=shape[2])
        elif len(shape) == 4:
            a = a.rearrange("p (a b c) -> p a b c", b=shape[2], c=shape[3])
        return a

    def wload(self, src, nel):
        s = self.wring_i.next()
        dst = self.wring[:, s, 0:nel]
        key = ("w", s)
        self.P.op("pool", lambda e: e.dma_start(out=dst, in_=src, max_dma_last_dim=4096),
                  writes=[key], dma="dw%d" % s, nobarrier=True)
        return dst, key

    def mm(self, out, lhsT, rhs, start, stop, reads, writes, inc=None, sgc=False):
        if inc is None:
            inc = stop
        self.P.op("pe", lambda e: e.matmul(out, lhsT=lhsT, rhs=rhs, start=start, stop=stop, skip_group_check=sgc),
                  reads=reads, writes=writes, inc=inc)

    def act(self, out, in_, func, reads, writes, bias=None, scale=None, ss=False):
        kw = {}
        if bias is not None:
            kw["bias"] = bias
        if scale is not None:
            kw["scale"] = scale
        self.P.op("act", lambda e: e.activation(out=out, in_=in_, func=func, **kw), reads=reads, writes=writes, ss=ss)

    def tt(self, out, in0, in1, op, reads, writes, eng="dve", ss=False):
        self.P.op(eng, lambda e: e.tensor_tensor(out=out, in0=in0, in1=in1, op=op), reads=reads, writes=writes, ss=ss)

    def ts(self, out, in0, s1, s2, op0, op1, reads, writes, eng="dve", ss=False):
        if op1 is None:
            self.P.op(eng, lambda e: e.tensor_scalar(out=out, in0=in0, scalar1=s1, scalar2=None, op0=op0),
                      reads=reads, writes=writes, ss=ss)
        else:
            self.P.op(eng, lambda e: e.tensor_scalar(out=out, in0=in0, scalar1=s1, scalar2=s2, op0=op0, op1=op1),
                      reads=reads, writes=writes, ss=ss)

    def stt(self, out, in0, scalar, in1, op0, op1, reads, writes, ss=False):
        self.P.op("dve", lambda e: e.scalar_tensor_tensor(out=out, in0=in0, scalar=scalar, in1=in1, op0=op0, op1=op1),
                  reads=reads, writes=writes, ss=ss)

    @staticmethod
    def bk(b):
        return [("ps", b)]

    def vcol(self, col):
        return self.vec[:, col:col + 1]

    def build(self):
        nc, P = self.nc, self.P
        with ExitStack() as es:
            sb = lambda name, shape, dtype: es.enter_context(nc.sbuf_tensor(name, shape, dtype))
            self.hT32 = sb("hT32", [128, 8, S], F32)
            self.hTb = sb("hTb", [128, 8, S], BF16)
            self.vec = sb("vec_sb", [128, NV], F32)
            self.dv = sb("dv", [128, 64], F32)
            self.dv2 = sb("dv2", [128, 64], F32)
            self.cf = sb("cf_sb", [128, NCF], F32)
            self.identb = sb("identb", [128, 128], BF16)
            self.onesb = sb("onesb", [128, 128], BF16)
            self.causb = sb("causb", [128, 2, 256], BF16)
            self.e8b = sb("e8b", [8, 8, 128], BF16)
            self.cpow = sb("cpow", [128, 2], F32)
            self.wring = sb("wring", [128, self.NSLOT, self.SLOT_ELEMS], BF16)
            self.wring_i = Ring(self.NSLOT)
            self.zring, self.tring, self.ybank = Ring(3), Ring(4), Ring(2)
            self.arena = sb("arena", [128, self.ARENA_BYTES // 4], F32)
            self.ps = [es.enter_context(nc.psum_tensor("ps%d" % i, [128, 512], F32)) for i in range(8)]

            P.same_engine_sync = True
            self.pending_tail = None
            self.setup()
            for ph in self.phases:
                kind, li = ph
                P.phase_barrier()
                if kind == "attn":
                    self.attn(li)
                elif kind == "lru":
                    self.lru(li)
                elif kind == "ffn":
                    self.ffn(li)
            if self.pending_tail is not None:
                self.zip_run([self.pending_tail])
                self.pending_tail = None
            ov = self.outT.rearrange("(c p) t -> p c t", p=128)
            toks = []
            for t in range(4):
                toks.append(P.op("sp", lambda e, t=t: e.dma_start(out=ov[:, :, t * 512:(t + 1) * 512], in_=self.hT32[:, :, t * 512:(t + 1) * 512]),
                                 reads=[("h32", c, t) for c in range(8)], dma="dout%d" % t))
            P.final_waits("sp", toks)
            P.emit(nc, es)
        return nc

    def setup(self):
        P = self.P
        h32keys = [("h32", c, t) for c in range(8) for t in range(4)]
        hbkeys = [("hb", c, t) for c in range(8) for t in range(4)]
        P.op("sp", lambda e: e.dma_start(out=self.vec[:], in_=self.vecs_d), writes=["cst"], dma="dc")
        P.op("sp", lambda e: e.dma_start(out=self.cf[:], in_=self.cf_d), writes=["cst"], dma="dc")
        P.op("pool", lambda e: e.dma_start(out=self.e8b[:].rearrange("p a b -> p (a b)"), in_=self.e8_d), writes=["cb"], dma="dg0")
        xv = self.xT.rearrange("(c p) t -> p c t", p=128)
        for t in range(4):
            P.op("sp", lambda e, t=t: e.dma_start(out=self.hT32[:, :, t * 512:(t + 1) * 512], in_=xv[:, :, t * 512:(t + 1) * 512]),
                 writes=[("h32", c, t) for c in range(8)], dma="dx%d" % t)
        for t in range(4):
            for c in range(8):
                tsl = slice(t * 512, (t + 1) * 512)
                if c % 2 == 0:
                    P.op("dve", lambda e, c=c, tsl=tsl: e.tensor_copy(self.hTb[:, c, tsl], self.hT32[:, c, tsl]),
                         reads=[("h32", c, t)], writes=[("hb", c, t)])
                else:
                    P.op("act", lambda e, c=c, tsl=tsl: e.copy(self.hTb[:, c, tsl], self.hT32[:, c, tsl]),
                         reads=[("h32", c, t)], writes=[("hb", c, t)])
        P.op("dve", lambda e: e.tensor_copy(self.identb[:], self.cf[:, C_ID:C_ID + 128]), reads=["cst"], writes=["cb"])
        P.op("dve", lambda e: e.tensor_copy(self.causb[:].rearrange("p a b -> p (a b)"), self.cf[:, C_CAUS:C_CAUS + 512]),
             reads=["cst"], writes=["cb"])
        P.op("dve", lambda e: e.memset(self.onesb[:], 1.0), writes=["cb"])
        P.op("dve", lambda e: e.memset(self.cpow[:, 0:1], -0.5), writes=["cb"])
        P.op("dve", lambda e: e.memset(self.cpow[:, 1:2], 0.5), writes=["cb"])
        lam = self.vec[:, V_LAM:V_LAM + 16]
        e_, z_, z2_, q_ = (self.dv2[:, i * 16:(i + 1) * 16] for i in range(4))
        self.act(e_, lam, AF.Exp, ["cst"], ["dv"], scale=-1.0)
        self.ts(z_, e_, 2.0, None, ALU.add, None, ["dv"], ["dv"])
        P.op("dve", lambda e: e.reciprocal(z_, z_), reads=["dv"], writes=["dv"])
        self.tt(z_, z_, e_, ALU.mult, ["dv"], ["dv"])
        self.tt(z2_, z_, z_, ALU.mult, ["dv"], ["dv"])
        self.ts(q_, z2_, 1.0 / 13.0, None, ALU.mult, None, ["dv"], ["dv"])
        for cc in (1.0 / 11, 1.0 / 9, 1.0 / 7, 1.0 / 5, 1.0 / 3):
            self.stt(q_, q_, cc, z2_, ALU.add, ALU.mult, ["dv"], ["dv"])
        self.stt(q_, q_, 1.0, z_, ALU.add, ALU.mult, ["dv"], ["dv"])
        self.ts(self.dv[:, 16:32], q_, -8.0, None, ALU.mult, None, ["dv"], ["dv"])
        self.ts(self.dv[:, 0:16], q_, -16.0, None, ALU.mult, None, ["dv"], ["dv"])
        self.ts(self.dv[:, 32:48], self.vec[:, V_BA:V_BA + 16], 0.5, None, ALU.mult, None, ["cst", "dv"], ["dv"])
        self.ts(self.dv[:, 48:64], self.vec[:, V_BX:V_BX + 16], 0.5, None, ALU.mult, None, ["cst", "dv"], ["dv"])

    @staticmethod
    def zip_run(gens, weights=None):
        gens = list(gens)
        weights = list(weights) if weights else [1] * len(gens)
        alive = [True] * len(gens)
        while any(alive):
            for i, g in enumerate(gens):
                for _ in range(weights[i]):
                    if not alive[i]:
                        break
                    try:
                        next(g)
                    except StopIteration:
                        alive[i] = False

    def out_proj_ln(self, tts, nk, wsrc, rhs_fn, rhs_keys_fn, ln_col, tmp_off):
        P = self.P
        zb = [self.view(tmp_off + i * 1024, [128, 512], BF16) for i in range(3)]
        zq = [self.view(tmp_off + 3072 + i * 1024, [128, 512], BF16) for i in range(3)]
        o = tmp_off + 6144
        mean = [self.view(o + i * 2048, [128, 512], F32) for i in range(2)]
        var = [self.view(o + 4096 + i * 2048, [128, 512], F32) for i in range(2)]
        tmp = [self.view(o + 8192 + i * 2048, [128, 512], F32) for i in range(4)]
        zring, tring = self.zring, self.tring
        sbank = [(6, 7), (2, 3)]
        ybank = self.ybank

        def mloop():
            pending = []

            def flush():
                for (m, ti, zi) in pending:
                    b1, b2 = sbank[ti]
                    self.mm(self.ps[b1][:], self.onesb[:], zb[zi][:], m == 0, m == 7, [("zb", zi), "cb"], self.bk(b1), inc=True)
                    self.mm(self.ps[b2][:], self.onesb[:], zq[zi][:], m == 0, m == 7, [("zq", zi), "cb"], self.bk(b2), inc=True)
                pending.clear()

            for m in range(8):
                w, wkey = self.wload(wsrc(m), nk * 128)
                for ti, tt in enumerate(tts):
                    yb = 4 + ybank.next()
                    tsl = slice(tt * 512, (tt + 1) * 512)
                    for k in range(nk):
                        self.mm(self.ps[yb][:], w[:, k * 128:(k + 1) * 128], rhs_fn(k, tt), k == 0, k == nk - 1,
                                [wkey] + rhs_keys_fn(k, tt), [("ps", yb)])
                    flush()
                    hk = ("h32", m, tt)
                    self.stt(self.hT32[:, m, tsl], self.hT32[:, m, tsl], ALPHA, self.ps[yb][:], ALU.mult, ALU.add,
                             [hk, ("ps", yb)], [hk])
                    zi = zring.next()
                    self.act(zb[zi][:], self.hT32[:, m, tsl], AF.Copy, [hk], [("zb", zi)])
                    self.act(zq[zi][:], self.hT32[:, m, tsl], AF.Square, [hk], [("zq", zi)])
                    pending.append((m, ti, zi))
                    yield
            flush()

        def tail(ti, tt):
            b1, b2 = sbank[ti]
            tsl = slice(tt * 512, (tt + 1) * 512)
            mk, vk = ("lnmean", ti), ("lnvar", ti)
            self.act(mean[ti][:], self.ps[b1][:], AF.Identity, self.bk(b1), [mk], scale=1.0 / D)
            yield
            self.act(var[ti][:], self.ps[b2][:], AF.Identity, self.bk(b2), [vk], scale=1.0 / D, bias=EPS)
            yield
            ti_ = tring.next()
            self.tt(tmp[ti_][:], mean[ti][:], mean[ti][:], ALU.mult, [mk], [("lntmp", ti_)])
            yield
            self.tt(var[ti][:], var[ti][:], tmp[ti_][:], ALU.subtract, [vk, ("lntmp", ti_)], [vk])
            yield
            self.act(var[ti][:], var[ti][:], AF.Sqrt, [vk], [vk])
            yield
            P.op("dve", lambda e: e.reciprocal(var[ti][:], var[ti][:]), reads=[vk], writes=[vk])
            yield
            self.tt(mean[ti][:], mean[ti][:], var[ti][:], ALU.mult, [mk, vk], [mk])
            for m in range(8):
                yield
                hk = ("h32", m, tt)
                ti_ = tring.next()
                tk = ("lntmp", ti_)
                self.tt(tmp[ti_][:], self.hT32[:, m, tsl], var[ti][:], ALU.mult, [hk, vk], [tk])
                yield
                self.tt(tmp[ti_][:], tmp[ti_][:], mean[ti][:], ALU.subtract, [tk, mk], [tk])
                g = self.vcol(V_LNG + ln_col * 8 + m)
                b = self.vcol(V_LNB + ln_col * 8 + m)
                self.act(self.hT32[:, m, tsl], tmp[ti_][:], AF.Identity, [tk, "cst"], [hk], scale=g, bias=b)
                self.act(self.hTb[:, m, tsl], tmp[ti_][:], AF.Identity, [tk, "cst"], [("hb", m, tt)], scale=g, bias=b)

        def tails():
            gens = [tail(ti, tt) for ti, tt in enumerate(tts)]
            while gens:
                for g_ in list(gens):
                    try:
                        next(g_)
                        yield
                    except StopIteration:
                        gens.remove(g_)

        return mloop(), tails()

    def mixer_out(self, nk, wsrc, rhs_fn, rhs_keys_fn, ln_col, tmp_off):
        m0, t0 = self.out_proj_ln([0, 1], nk, wsrc, rhs_fn, rhs_keys_fn, ln_col, tmp_off)
        m1, t1 = self.out_proj_ln([2, 3], nk, wsrc, rhs_fn, rhs_keys_fn, ln_col, tmp_off)
        self.zip_run([m0])
        self.zip_run([t0, m1], [3, 1])
        self.P.snapshot_early()
        self.pending_tail = t1

    def ffn(self, li):
        P = self.P
        ACT_OFF = 0
        SG_OFF = 45056
        TMP_OFF = 49152
        actT = self.view(ACT_OFF, [128, NF, 1024], BF16)
        sg = [self.view(SG_OFF + i * 2048, [128, 512], F32) for i in range(2)]
        sgr = Ring(2)
        gb, ub = Ring(2), Ring(2)

        def inproj(tts):
            for f in range(NF):
                w, wkey = self.wload(self.fwin[li, f], 2048)
                for ti, tt in enumerate(tts):
                    g = gb.next()
                    u = 2 + ub.next()
                    tsl = slice(tt * 512, (tt + 1) * 512)
                    for k in range(8):
                        self.mm(self.ps[g][:], w[:, k * 128:(k + 1) * 128], self.hTb[:, k, tsl], k == 0, k == 7,
                                [wkey, ("hb", k, tt)], [("ps", g)])
                    for k in range(8):
                        self.mm(self.ps[u][:], w[:, 1024 + k * 128:1024 + (k + 1) * 128], self.hTb[:, k, tsl], k == 0, k == 7,
                                [wkey, ("hb", k, tt)], [("ps", u)])
                    si = sgr.next()
                    self.act(sg[si][:], self.ps[g][:], AF.Silu, [("ps", g)], [("sg", si)])
                    self.tt(actT[:, f, ti * 512:(ti + 1) * 512], sg[si][:], self.ps[u][:], ALU.mult,
                            [("sg", si), ("ps", u)], [("actT", f, ti)])
                    yield

        parts = []
        for grp in range(2):
            tts = [2 * grp, 2 * grp + 1]
            parts.append(self.out_proj_ln(tts, NF, lambda m: self.fwout[li, m],
                                          lambda k, tt: actT[:, k, (tt % 2) * 512:(tt % 2 + 1) * 512],
                                          lambda k, tt: [("actT", k, tt % 2)], li * 2 + 1, TMP_OFF))
        P.default_early = True
        tail, self.pending_tail = self.pending_tail, None
        if tail is not None:
            self.zip_run([tail, inproj([0, 1])], [2, 1])
        else:
            self.zip_run([inproj([0, 1])])
        P.default_early = False
        P.phase_barrier()
        self.zip_run([parts[0][0]])
        self.zip_run([parts[0][1], inproj([2, 3])], [2, 1])
        self.zip_run([parts[1][0]])
        P.snapshot_early()
        self.pending_tail = parts[1][1]

    def attn(self, li):
        P = self.P
        j = li // 2
        QT = [self.view(0 + i * 12288, [128, S], BF16) for i in range(2)]
        KT = [self.view(4096 + i * 12288, [128, S], BF16) for i in range(2)]
        VV = [self.view(8192 + i * 12288, [128, 16, 128], BF16) for i in range(2)]
        OT_OFF = 24576
        oT = self.view(OT_OFF, [128, 8, S], BF16)
        o = OT_OFF + 32768
        PT = [self.view(o + i * 512, [128, 256], BF16) for i in range(4)]
        o += 4 * 512
        lsT = self.view(o, [8, 2, S], BF16)
        o += 8192
        gm = self.view(o, [128, 128], F32); o += 512
        cmp_ = self.view(o, [128, 1024], F32); o += 4096
        rank = self.view(o, [128, 128], F32); o += 512
        lsel = [self.view(o + i * 512, [128, 128], F32) for i in range(2)]; o += 1024
        ksum = self.view(o, [128, 8], F32); o += 32
        kmT = [self.view(o + i * 32, [128, 8], BF16) for i in range(2)]; o += 64
        rden = [self.view(o + i * 1024, [128, 256], F32) for i in range(2)]; o += 2048
        assert o <= self.ARENA_BYTES
        TMP_OFF = 57344
        projb = Ring(2)
        ptr = Ring(4)
        sbk = Ring(2)
        rdr = Ring(2)
        past = self.cf[:, C_PAST:C_PAST + 128]

        def proj_g(h):
            hb = h % 2
            w, wkey = self.wload(self.wqkv[j, h], 3072)
            for tt in range(4):
                b = projb.next()
                tsl = slice(tt * 512, (tt + 1) * 512)
                for k in range(8):
                    self.mm(self.ps[b][:], w[:, k * 128:(k + 1) * 128], self.hTb[:, k, tsl], k == 0, k == 7,
                            [wkey, ("hb", k, tt)], [("ps", b)])
                self.act(QT[hb][:, tsl], self.ps[b][:], AF.Copy, [("ps", b)], [("QT", hb, tt)])
                yield
            for tt in range(4):
                b = projb.next()
                tsl = slice(tt * 512, (tt + 1) * 512)
                for k in range(8):
                    self.mm(self.ps[b][:], w[:, 1024 + k * 128:1024 + (k + 1) * 128], self.hTb[:, k, tsl], k == 0, k == 7,
                            [wkey, ("hb", k, tt)], [("ps", b)])
                P.op("dve", lambda e, b=b, tt=tt: e.tensor_reduce(
                    out=ksum[:, 2 * tt:2 * tt + 2], in_=self.ps[b][:].rearrange("p (a b) -> p a b", b=256),
                    axis=AX.X, op=ALU.add), reads=[], writes=[("ksum", tt), ("ps", b)])
                self.act(KT[hb][:, tsl], self.ps[b][:], AF.Copy, [], [("KT", hb, tt), ("ps", b)])
                yield
            self.ts(kmT[hb][:], ksum[:], 1.0 / 256, None, ALU.mult, None, [("ksum", t) for t in range(4)], [("kmT", hb)], ss=True)
            for g4 in range(4):
                b = projb.next()
                for t4 in range(4):
                    t16 = g4 * 4 + t4
                    for k in range(8):
                        self.mm(self.ps[b][:, t4 * 128:(t4 + 1) * 128], self.hTb[:, k, t16 * 128:(t16 + 1) * 128],
                                w[:, 2048 + k * 128:2048 + (k + 1) * 128], k == 0, k == 7,
                                [wkey, ("hb", k, t16 // 4)], [("ps", b)])
                P.op("dve", lambda e, b=b, g4=g4: e.tensor_copy(
                    VV[hb][:, g4 * 4:(g4 + 1) * 4, :].rearrange("p a b -> p (a b)"), self.ps[b][:]),
                    reads=[("ps", b)], writes=[("V", hb, g4)])
                yield

        def proj(h):
            for _ in proj_g(h):
                pass

        def gate_a(h):
            hb = h % 2
            for t in range(16):
                self.mm(self.ps[2][:, t * 8:(t + 1) * 8], QT[hb][:, t * 128:(t + 1) * 128], kmT[hb][:], True, True,
                        [("QT", hb, t // 4), ("kmT", hb)], [("ps", 2)], inc=(t == 15))
            self.tt(gm[:], self.ps[2][:, 0:128], past, ALU.add, [("ps", 2), "cst"], ["gm"], ss=True)
            g3 = gm[:].rearrange("p (t n) -> p t n", n=8)
            in0 = g3.unsqueeze(2).broadcast_to([128, 16, 8, 8])
            in1 = g3.unsqueeze(3).broadcast_to([128, 16, 8, 8])
            self.tt(cmp_[:].rearrange("p (t n m) -> p t n m", n=8, m=8), in0, in1, ALU.is_gt, ["gm"], ["cmp"], ss=True)
            P.op("dve", lambda e: e.tensor_reduce(out=rank[:], in_=cmp_[:].rearrange("p (a m) -> p a m", m=8),
                                                  axis=AX.X, op=ALU.add), reads=["cmp"], writes=["rank"], ss=True)
            self.ts(lsel[hb][:], rank[:], 2.5, NEG, ALU.is_gt, ALU.mult, ["rank"], [("lsel", hb)], ss=True)

        def gate_b(h):
            hb = h % 2
            for g4 in range(4):
                for t4 in range(4):
                    t = g4 * 4 + t4
                    P.op("pe", lambda e, t=t, t4=t4: e.transpose(self.ps[3][0:8, t4 * 128:(t4 + 1) * 128],
                                                                 lsel[hb][:, t * 8:(t + 1) * 8], self.cf[:, C_ID:C_ID + 128]),
                         reads=[("lsel", hb), "cst"], writes=[("ps", 3)], inc=(t4 == 3))
                self.act(lsT[0:8, hb, g4 * 512:(g4 + 1) * 512], self.ps[3][0:8, :], AF.Copy, [("ps", 3)], [("lsT", hb, g4)])

        def attention(h):
            hb = h % 2
            for qb in range(8):
                ob = 6 + qb % 2
                qsl = slice(qb * 256, (qb + 1) * 256)
                nkb = qb + 1
                pend = None

                def pv(kb, pslots, first, last):
                    for jj in range(2):
                        jt = 2 * kb + jj
                        st, sp_ = first and jj == 0, last and jj == 1
                        self.mm(self.ps[ob][:, 0:256], VV[hb][:, jt, :], PT[pslots[jj]][:], st, sp_,
                                [("V", hb, jt // 4), ("PT", pslots[jj])], [("ps", ob)], inc=True, sgc=True)
                        self.mm(self.ps[ob][:, 256:512], self.onesb[:], PT[pslots[jj]][:], False, sp_,
                                [("PT", pslots[jj]), "cb"], [("ps", ob)], inc=True, sgc=True)

                for kb in range(nkb):
                    sb_ = 4 + sbk.next()
                    slots = []
                    for jj in range(2):
                        jt = 2 * kb + jj
                        csl = slice(jj * 256, (jj + 1) * 256)
                        nomask = kb < qb and qb <= 3
                        self.mm(self.ps[sb_][:, csl], KT[hb][:, jt * 128:(jt + 1) * 128], QT[hb][:, qsl], True, nomask,
                                [("KT", hb, jt // 4), ("QT", hb, qb // 2)], [("ps", sb_)], inc=nomask)
                        if nomask:
                            pass
                        elif kb < qb:
                            self.mm(self.ps[sb_][:, csl], self.e8b[0:8, kb, :], lsT[0:8, hb, qsl], False, True,
                                    ["cb", ("lsT", hb, qb // 2)], [("ps", sb_)], inc=True)
                        else:
                            self.mm(self.ps[sb_][:, csl], self.identb[:], self.causb[:, jj, :], False, True,
                                    ["cb"], [("ps", sb_)], inc=True)
                    for jj in range(2):
                        jt = 2 * kb + jj
                        csl = slice(jj * 256, (jj + 1) * 256)
                        pi = ptr.next()
                        slots.append(pi)
                        r = jt - 2 * qb - 1 + 16
                        bias = self.cf[:, C_BIAS + h * 17 + r:C_BIAS + h * 17 + r + 1]
                        self.act(PT[pi][:], self.ps[sb_][:, csl], AF.Exp, [("ps", sb_), "cst"], [("PT", pi)],
                                 bias=bias, scale=SCALE)
                    if pend is not None:
                        pv(pend[0], pend[1], pend[0] == 0, False)
                    pend = (kb, slots)
                pv(pend[0], pend[1], pend[0] == 0, True)
                ri = rdr.next()
                P.op("dve", lambda e, ri=ri, ob=ob: e.reciprocal(rden[ri][:], self.ps[ob][:, 256:512]),
                     reads=[("ps", ob)], writes=[("rden", ri)])
                self.tt(oT[:, h, qsl], self.ps[ob][:, 0:256], rden[ri][:], ALU.mult,
                        [("ps", ob), ("rden", ri)], [("oT", h, qb // 2)], ss=True)

        P.default_early = True
        tail, self.pending_tail = self.pending_tail, None
        if tail is not None:
            self.zip_run([tail])
        proj(0)
        P.default_early = False
        P.phase_barrier()
        gate_a(0)
        gate_b(0)
        for h in range(NH):
            if h + 1 < NH:
                proj(h + 1)
                gate_a(h + 1)
            attention(h)
            if h + 1 < NH:
                gate_b(h + 1)
        self.mixer_out(8, lambda m: self.wo[j, m], lambda k, tt: oT[:, k, tt * 512:(tt + 1) * 512],
                       lambda k, tt: [("oT", k, tt)], li * 2, TMP_OFF)

    def lru(self, li):
        P = self.P
        j = li // 2
        if self.pending_tail is not None:
            tail, self.pending_tail = self.pending_tail, None
            P.default_early = True
            self.zip_run([tail])
            P.default_early = False
            P.phase_barrier()
        HS = 1024
        mT = self.view(0, [128, 8, S], BF16)
        o = 32768
        A2 = [self.view(o + i * 2048, [128, 512], F32) for i in range(2)]; o += 4096
        OM2 = [self.view(o + i * 2048, [128, 512], F32) for i in range(2)]; o += 4096
        IX2 = [self.view(o + i * 2048, [128, 512], F32) for i in range(2)]; o += 4096
        GT2 = [self.view(o + i * 2048, [128, 512], F32) for i in range(2)]; o += 4096
        xpad = [self.view(o + i * 2064, [128, 516], F32) for i in range(3)]; o += 6192
        xc = [self.view(o + i * 2048, [128, 512], F32) for i in range(4)]; o += 8192
        xcb = [self.view(o + i * 1024, [128, 512], BF16) for i in range(2)]; o += 2048
        t1 = [self.view(o + i * 2048, [128, 512], F32) for i in range(6)]; o += 12288
        U = [self.view(o + i * 2048, [128, 512], F32) for i in range(2)]; o += 4096
        wab = self.view(o, [128, 8, 128], BF16); o += 2048
        wxb = self.view(o, [128, 8, 128], BF16); o += 2048
        carry = self.view(o, [128, 2], F32); o += 8
        assert o <= self.ARENA_BYTES, o
        TMP_OFF = 57344
        P.op("pool", lambda e: e.dma_start(out=wab[:].rearrange("p a b -> p (a b)"), in_=self.lwa[j], max_dma_last_dim=4096),
             writes=["wab"], dma="dga")
        P.op("pool", lambda e: e.dma_start(out=wxb[:].rearrange("p a b -> p (a b)"), in_=self.lwx[j], max_dma_last_dim=4096),
             writes=["wxb"], dma="dgb")
        xbk, ybk = Ring(2), Ring(2)
        xpr, xcr, xcbr, t1r, ur = Ring(3), Ring(4), Ring(2), Ring(6), Ring(2)
        allk = lambda n: [(n, t) for t in range(2)]
        st = {}
        cur = {"w": None, "prev_xp": None}

        def consts(c):
            return dict(cw=[self.vcol(V_CW + (j * 4 + tap) * 8 + c) for tap in range(4)],
                        cb=self.vcol(V_CB + j * 8 + c),
                        hcl=self.dv[:, 16 + j * 8 + c:16 + j * 8 + c + 1],
                        hba=self.dv[:, 32 + j * 8 + c:32 + j * 8 + c + 1],
                        hbx=self.dv[:, 48 + j * 8 + c:48 + j * 8 + c + 1])

        def s1(n, c, tt, xi, pxi):
            w, wkey = cur["w"]
            k_ = consts(c)
            tsl = slice(tt * 512, (tt + 1) * 512)
            xb_ = xbk.next()
            yb_ = 2 + ybk.next()
            ci = xcr.next()
            bi = xcbr.next()
            rb, ib = 4 + 2 * (n % 2), 5 + 2 * (n % 2)
            st[n] = dict(yb=yb_, ci=ci, rb=rb, ib=ib)
            for k in range(8):
                self.mm(self.ps[xb_][:], w[:, k * 128:(k + 1) * 128], self.hTb[:, k, tsl], k == 0, k == 7,
                        [wkey, ("hb", k, tt)], [("ps", xb_)])
            for k in range(8):
                self.mm(self.ps[yb_][:], w[:, 1024 + k * 128:1024 + (k + 1) * 128], self.hTb[:, k, tsl], k == 0, k == 7,
                        [wkey, ("hb", k, tt)], [("ps", yb_)])
            yield
            xk = ("xpad", xi)
            self.act(xpad[xi][:, 3:515], self.ps[xb_][:], AF.Copy, [("ps", xb_)], [xk])
            ck = ("xc", ci)
            self.act(xc[ci][:], self.ps[xb_][:], AF.Identity, [("ps", xb_), "cst"], [ck], scale=k_["cw"][3], bias=k_["cb"])
            yield
            if tt == 0:
                P.op("dve", lambda e: e.memset(xpad[xi][:, 0:3], 0.0), writes=[xk])
            else:
                P.op("dve", lambda e: e.tensor_copy(xpad[xi][:, 0:3], xpad[pxi][:, 512:515]),
                     reads=[("xpad", pxi)], writes=[xk])
            for tap in (2, 1, 0):
                yield
                self.stt(xc[ci][:], xpad[xi][:, tap:tap + 512], k_["cw"][tap], xc[ci][:], ALU.mult, ALU.add,
                         [xk, ck, "cst"], [ck])
            yield
            self.act(xcb[bi][:], xc[ci][:], AF.Copy, [ck], [("xcb", bi)])
            yield
            self.mm(self.ps[rb][:], wab[:, c, :], xcb[bi][:], True, True, ["wab", ("xcb", bi)], [("ps", rb)])
            self.mm(self.ps[ib][:], wxb[:, c, :], xcb[bi][:], True, True, ["wxb", ("xcb", bi)], [("ps", ib)])

        def s2(n, c, tt):
            k_ = consts(c)
            d = st.pop(n)
            yb_, ci, rb, ib = d["yb"], d["ci"], d["rb"], d["ib"]
            ck = ("xc", ci)
            tl = n % 2
            lsl = slice(0, 512)
            A_, OM, IX, GT = A2[tl], OM2[tl], IX2[tl], GT2[tl]
            ta, tb, tg, ua = t1r.next(), t1r.next(), t1r.next(), ur.next()
            tk, tbk, tgk, uk = ("t1", ta), ("t1", tb), ("t1", tg), ("U", ua)
            self.act(t1[ta][:], self.ps[rb][:], AF.Tanh, [("ps", rb), "dv"], [tk], scale=0.5, bias=k_["hba"])
            yield
            self.act(U[ua][:], t1[ta][:], AF.Identity, [tk, "dv"], [uk], scale=k_["hcl"], bias=k_["hcl"])
            yield
            self.act(t1[ta][:], U[ua][:], AF.Identity, [uk], [tk], scale=1.0 / 24.0, bias=1.0 / 6.0)
            yield
            self.act(t1[tb][:], self.ps[ib][:], AF.Tanh, [("ps", ib), "dv"], [tbk], scale=0.5, bias=k_["hbx"])
            yield
            self.act(GT[:, lsl], self.ps[yb_][:], AF.Gelu_apprx_tanh, [("ps", yb_)], [("GT", tl)])
            for cc in (None, 0.5, 1.0):
                yield
                if cc is None:
                    self.tt(t1[ta][:], t1[ta][:], U[ua][:], ALU.mult, [tk, uk], [tk])
                else:
                    self.stt(t1[ta][:], t1[ta][:], cc, U[ua][:], ALU.add, ALU.mult, [tk, uk], [tk])
            yield
            self.stt(U[ua][:], t1[ta][:], 2.0, t1[ta][:], ALU.add, ALU.mult, [tk], [uk])
            yield
            self.stt(IX[:, lsl], t1[tb][:], 1.0, xc[ci][:], ALU.add, ALU.mult, [tbk, ck], [("IX", tl)])
            yield
            self.ts(A_[:, lsl], t1[ta][:], 1.0, None, ALU.add, None, [tk], [("A", tl)])
            yield
            self.ts(OM[:, lsl], U[ua][:], -1.0, 0.0, ALU.mult, ALU.max, [uk], [("OM", tl)])

        def s3(n, c, tt):
            tl = n % 2
            A_, OM, IX, GT = A2[tl], OM2[tl], IX2[tl], GT2[tl]
            tsl = slice(tt * 512, (tt + 1) * 512)
            self.act(OM[:], OM[:], AF.Sqrt, [("OM", tl)], [("OM", tl)])
            yield
            yield
            self.stt(IX[:], OM[:], 0.5, IX[:], ALU.mult, ALU.mult, [("OM", tl), ("IX", tl)], [("IX", tl)])
            yield
            yield
            init = 0.0 if tt == 0 else carry[:, 0:1]
            P.op("dve", lambda e: e.tensor_tensor_scan(out=OM[:], data0=A_[:], data1=IX[:], initial=init,
                                                       op0=ALU.mult, op1=ALU.add),
                 reads=[("A", tl), ("IX", tl), "carry"], writes=[("OM", tl)])
            if tt < 3:
                P.op("pool", lambda e: e.tensor_copy(carry[:, 0:1], OM[:, 511:512]), reads=[("OM", tl)], writes=["carry"])
            self.tt(mT[:, c, tsl], OM[:], GT[:], ALU.mult, [("OM", tl), ("GT", tl)], [("mT", c, tt)], eng="pool")

        def zip_run(gens):
            gens = list(gens)
            while gens:
                for g in list(gens):
                    try:
                        next(g)
                    except StopIteration:
                        gens.remove(g)

        tiles = [(c, tt) for c in range(8) for tt in range(4)]
        NT_ = len(tiles)
        for n in range(NT_ + 2):
            gens = []
            if n < NT_:
                c, tt = tiles[n]
                if tt == 0:
                    cur["w"] = self.wload(self.lwin[j, c], 2048)
                xi = xpr.next()
                pxi = cur["prev_xp"]
                cur["prev_xp"] = xi
                gens.append(s1(n, c, tt, xi, pxi))
            if 1 <= n <= NT_:
                gens.append(s2(n - 1, *tiles[n - 1]))
            if n >= 2:
                gens.append(s3(n - 2, *tiles[n - 2]))
            zip_run(gens)
        if self.dbg == "lru_mT":
            for c in range(8):
                tok = P.op("pool", lambda e, c=c: e.dma_start(out=self.dbgT[c * 128:(c + 1) * 128, :], in_=mT[:, c, :], max_dma_last_dim=4096),
                           reads=[("mT", c, t) for t in range(4)], dma="ddbg")
            P.final_waits("pool", [tok])
            return
        self.mixer_out(8, lambda m: self.lwout[j, m], lambda k, tt: mT[:, k, tt * 512:(tt + 1) * 512],
                       lambda k, tt: [("mT", k, tt)], li * 2, TMP_OFF)


def _consts():
    cf = np.zeros((128, NCF), np.float32)
    cf[:, C_ID:C_ID + 128] = np.eye(128, dtype=np.float32)
    p = np.arange(128, dtype=np.float64)
    for h in range(NH):
        slope = 2.0 ** (-8.0 * (h + 1) / NH)
        for r in range(17):
            cf[:, C_BIAS + h * 17 + r] = slope * (p + 128.0 * (r - 16))
    past = np.zeros((16, 8), np.float32)
    for t in range(16):
        for n in range(8):
            if n >= t // 2:
                past[t, n] = -1e30
    cf[:, C_PAST:C_PAST + 128] = past.reshape(1, 128)
    q = np.arange(256)
    for half in range(2):
        k = half * 128 + np.arange(128)
        cf[:, C_CAUS + half * 256:C_CAUS + (half + 1) * 256] = np.where(q[None, :] >= k[:, None], 0.0, NEG)
    e8 = np.zeros((8, 8, 128), np.float32)
    for kb in range(8):
        e8[kb, kb, :] = 1.0
    return cf, e8.reshape(8, 1024)


def _prep_weights(inp):
    f = lambda a: np.ascontiguousarray(a, dtype=np.float32)
    out = {}
    w = inp["attn_w_qkv"].reshape(2, 8, 128, 3, NH, 128)
    out["wqkv"] = f(w.transpose(0, 4, 2, 3, 1, 5).reshape(2, NH, 128, 3072))
    w = inp["attn_w_o"].reshape(2, 8, 128, 8, 128)
    out["wo"] = f(w.transpose(0, 3, 2, 1, 4).reshape(2, 8, 128, 1024))
    w = inp["lru_w_in"].reshape(2, 8, 128, 2, 8, 128)
    out["lwin"] = f(w.transpose(0, 4, 2, 3, 1, 5).reshape(2, 8, 128, 2048))
    w = inp["lru_w_out"].reshape(2, 8, 128, 8, 128)
    out["lwout"] = f(w.transpose(0, 3, 2, 1, 4).reshape(2, 8, 128, 1024))
    out["lwa"] = f(inp["lru_w_a"].transpose(0, 2, 1, 3).reshape(2, 128, 1024))
    out["lwx"] = f(inp["lru_w_x"].transpose(0, 2, 1, 3).reshape(2, 128, 1024))
    w = inp["ffn_w_in"].reshape(DEPTH, 8, 128, 2, NF, 128)
    out["fwin"] = f(w.transpose(0, 4, 2, 3, 1, 5).reshape(DEPTH, NF, 128, 2048))
    w = inp["ffn_w_out"].reshape(DEPTH, NF, 128, 8, 128)
    out["fwout"] = f(w.transpose(0, 3, 2, 1, 4).reshape(DEPTH, 8, 128, FF))
    vecs = np.zeros((128, NV), np.float32)
    pc = lambda a: a.reshape(a.shape[:-1] + (8, 128))
    vecs[:, V_LNG:V_LNG + 64] = pc(inp["ln_g"]).transpose(3, 0, 1, 2).reshape(128, 64)
    vecs[:, V_LNB:V_LNB + 64] = pc(inp["ln_b"]).transpose(3, 0, 1, 2).reshape(128, 64)
    vecs[:, V_CW:V_CW + 64] = pc(inp["lru_conv_w"]).transpose(3, 0, 1, 2).reshape(128, 64)
    vecs[:, V_CB:V_CB + 16] = pc(inp["lru_conv_b"]).transpose(2, 0, 1).reshape(128, 16)
    vecs[:, V_BA:V_BA + 16] = pc(inp["lru_b_a"]).transpose(2, 0, 1).reshape(128, 16)
    vecs[:, V_BX:V_BX + 16] = pc(inp["lru_b_x"]).transpose(2, 0, 1).reshape(128, 16)
    vecs[:, V_LAM:V_LAM + 16] = pc(inp["lru_lambda"]).transpose(2, 0, 1).reshape(128, 16)
    out["vecs"] = vecs
    out["cf"], out["e8"] = _consts()
    return out


ALL_PHASES = [("attn", 0), ("ffn", 0), ("lru", 1), ("ffn", 1), ("attn", 2), ("ffn", 2), ("lru", 3), ("ffn", 3)]
_NC_CACHE = {}


def run(inputs, phases=None, trace=False, dbg=None):
    phases = ALL_PHASES if phases is None else phases
    key = (tuple(phases), dbg)
    if key not in _NC_CACHE:
        _NC_CACHE[key] = K(phases, dbg).build()
    nc = _NC_CACHE[key]
    inp = {k: np.asarray(v) for k, v in inputs.items()}
    shared = _prep_weights(inp)
    x = np.asarray(inp["x"], dtype=np.float32)
    in_maps = []
    for b in range(8):
        m = dict(shared)
        m["xT"] = np.ascontiguousarray(x[b].T)
        in_maps.append(m)
    res = run_bass_kernel_spmd(nc, in_maps, core_ids=list(range(8)), trace=trace)
    out = np.stack([np.ascontiguousarray(r["outT"].T) for r in res.results], axis=0).astype(np.float32)
    if dbg:
        out = np.stack([np.ascontiguousarray(r["dbgT"].T) for r in res.results], axis=0).astype(np.float32)
    return out, res


def kernel(**inputs):
    out, _ = run(inputs)
    return out
```

```python
import numpy as np
from contextlib import ExitStack
import concourse.bass as bass
import concourse.mybir as mybir
from concourse.bass_utils import run_bass_kernel_spmd

F32 = mybir.dt.float32
BF16 = mybir.dt.bfloat16
AF = mybir.ActivationFunctionType
ALU = mybir.AluOpType
AX = mybir.AxisListType

D = 1024
S = 2048
DEPTH = 4
NH = 8
FF = 2816
NF = FF // 128
ALPHA = float((2 * DEPTH) ** 0.25)
EPS = 1e-5
SCALE = float(128 ** -0.5)
NEG = -30000.0
ENGS = ["pe", "act", "dve", "pool", "sp"]

V_LNG = 0
V_LNB = 64
V_CW = 128
V_CB = 192
V_BA = 208
V_BX = 224
V_LAM = 240
NV = 256
C_ID = 0
C_BIAS = 128
C_PAST = 264
C_CAUS = 392
NCF = 904


class Prog:
    def __init__(self, same_engine_sync=False):
        self.ops = {e: [] for e in ENGS}
        self.cnt = {e: 0 for e in ENGS}
        self.dcnt = {}
        self.res = {}
        self.waited = {e: {} for e in ENGS}
        self.same_engine_sync = same_engine_sync
        self.barrier = {}
        self.early = {}
        self.early_next = None
        self.default_early = False

    def _counts(self):
        return {"e_" + e: self.cnt[e] for e in ENGS if self.cnt[e] > 0}

    def snapshot_early(self):
        self.early_next = self._counts()

    def phase_barrier(self):
        self.barrier = self._counts()
        self.early = self.early_next if self.early_next is not None else self.barrier
        self.early_next = None

    def _st(self, key):
        st = self.res.get(key)
        if st is None:
            st = {"w": None, "r": {}}
            self.res[key] = st
        return st

    def op(self, eng, fn, reads=(), writes=(), dma=None, inc=True, nobarrier=False, ss=False):
        need = {} if nobarrier else dict(self.early if self.default_early else self.barrier)

        def add(tok):
            if tok is None:
                return
            s, v = tok
            if need.get(s, 0) < v:
                need[s] = v

        for r in reads:
            add(self._st(r)["w"])
        for w in writes:
            st = self._st(w)
            add(st["w"])
            for s, v in st["r"].items():
                add((s, v))
        if dma is None:
            if inc:
                self.cnt[eng] += 1
                tok = ("e_" + eng, self.cnt[eng])
                inc = 1
            else:
                tok = ("e_" + eng, self.cnt[eng] + 1)
                inc = 0
        else:
            self.dcnt[dma] = self.dcnt.get(dma, 0) + 16
            tok = (dma, self.dcnt[dma])
            inc = 16
        waits = []
        wd = self.waited[eng]
        for s, v in need.items():
            if s == "e_" + eng and (eng == "pe" or not (self.same_engine_sync or ss)):
                continue
            if wd.get(s, 0) >= v:
                continue
            wd[s] = v
            waits.append((s, v))
        self.ops[eng].append((fn, waits, tok[0], inc))
        for r in reads:
            st = self._st(r)
            if st["r"].get(tok[0], 0) < tok[1]:
                st["r"][tok[0]] = tok[1]
        for w in writes:
            st = self._st(w)
            st["w"] = tok
            st["r"] = {}
        return tok

    def final_waits(self, eng, toks):
        self.ops[eng].append((None, list(toks), None, 0))

    def emit(self, nc, es):
        sems = {}
        names = ["e_" + e for e in ENGS if self.cnt[e] > 0] + list(self.dcnt.keys())
        for n in names:
            sems[n] = es.enter_context(nc.semaphore(n))
        block = es.enter_context(nc.Block())
        engmap = {"pe": block.tensor, "act": block.scalar, "dve": block.vector,
                  "pool": block.gpsimd, "sp": block.sync}
        for e in ENGS:
            ops = self.ops[e]
            if not ops:
                continue

            def body(eng, ops=ops):
                for fn, waits, sname, inc in ops:
                    for s, v in waits:
                        eng.wait_ge(sems[s], v)
                    if fn is not None:
                        ins = fn(eng)
                        if inc:
                            ins.then_inc(sems[sname], inc)

            engmap[e](body)


class Ring:
    def __init__(self, n):
        self.n = n
        self.i = -1

    def next(self):
        self.i = (self.i + 1) % self.n
        return self.i


class K:
    NSLOT = 3
    SLOT_ELEMS = 3072
    ARENA_BYTES = 85 * 1024

    def __init__(self, phases, dbg=None):
        self.phases = phases
        self.dbg = dbg
        self.nc = nc = bass.Bass("TRN2", target_bir_lowering=False)
        self.P = Prog()
        dt = lambda name, shape, kind="ExternalInput": nc.dram_tensor(name, shape, F32, kind=kind).ap()
        self.xT = dt("xT", [D, S])
        self.outT = dt("outT", [D, S], "ExternalOutput")
        self.wqkv = dt("wqkv", [2, NH, 128, 3072])
        self.wo = dt("wo", [2, 8, 128, 1024])
        self.lwin = dt("lwin", [2, 8, 128, 2048])
        self.lwout = dt("lwout", [2, 8, 128, 1024])
        self.lwa = dt("lwa", [2, 128, 1024])
        self.lwx = dt("lwx", [2, 128, 1024])
        self.fwin = dt("fwin", [DEPTH, NF, 128, 2048])
        self.fwout = dt("fwout", [DEPTH, 8, 128, FF])
        self.vecs_d = dt("vecs", [128, NV])
        self.cf_d = dt("cf", [128, NCF])
        self.e8_d = dt("e8", [8, 1024])
        if dbg:
            self.dbgT = dt("dbgT", [D, S], "ExternalOutput")

    def view(self, off, shape, dtype):
        nel = int(np.prod(shape[1:]))
        nbytes = nel * (4 if dtype == F32 else 2)
        assert off % 4 == 0 and off + nbytes <= self.ARENA_BYTES, (off, nbytes)
        a = self.arena[0:shape[0], off // 4:(off + nbytes + 3) // 4]
        if dtype != F32:
            a = a.bitcast(dtype)
        if len(shape) == 3:
            a = a.rearrange("p (a b) -> p a b", b=shape[2])
        elif len(shape) == 4:
            a = a.rearrange("p (a b c) -> p a b c", b=shape[2], c=shape[3])
        return a

    def wload(self, src, nel):
        s = self.wring_i.next()
        dst = self.wring[:, s, 0:nel]
        key = ("w", s)
        self.P.op("pool", lambda e: e.dma_start(out=dst, in_=src, max_dma_last_dim=4096),
                  writes=[key], dma="dw%d" % s, nobarrier=True)
        return dst, key

    def mm(self, out, lhsT, rhs, start, stop, reads, writes, inc=None, sgc=False):
        if inc is None:
            inc = stop
        self.P.op("pe", lambda e: e.matmul(out, lhsT=lhsT, rhs=rhs, start=start, stop=stop, skip_group_check=sgc),
                  reads=reads, writes=writes, inc=inc)

    def act(self, out, in_, func, reads, writes, bias=None, scale=None, ss=False):
        kw = {}
        if bias is not None:
            kw["bias"] = bias
        if scale is not None:
            kw["scale"] = scale
        self.P.op("act", lambda e: e.activation(out=out, in_=in_, func=func, **kw), reads=reads, writes=writes, ss=ss)

    def tt(self, out, in0, in1, op, reads, writes, eng="dve", ss=False):
        self.P.op(eng, lambda e: e.tensor_tensor(out=out, in0=in0, in1=in1, op=op), reads=reads, writes=writes, ss=ss)

    def ts(self, out, in0, s1, s2, op0, op1, reads, writes, eng="dve", ss=False):
        if op1 is None:
            self.P.op(eng, lambda e: e.tensor_scalar(out=out, in0=in0, scalar1=s1, scalar2=None, op0=op0),
                      reads=reads, writes=writes, ss=ss)
        else:
            self.P.op(eng, lambda e: e.tensor_scalar(out=out, in0=in0, scalar1=s1, scalar2=s2, op0=op0, op1=op1),
                      reads=reads, writes=writes, ss=ss)

    def stt(self, out, in0, scalar, in1, op0, op1, reads, writes, ss=False):
        self.P.op("dve", lambda e: e.scalar_tensor_tensor(out=out, in0=in0, scalar=scalar, in1=in1, op0=op0, op1=op1),
                  reads=reads, writes=writes, ss=ss)

    @staticmethod
    def bk(b):
        return [("ps", b)]

    def vcol(self, col):
        return self.vec[:, col:col + 1]

    def build(self):
        nc, P = self.nc, self.P
        with ExitStack() as es:
            sb = lambda name, shape, dtype: es.enter_context(nc.sbuf_tensor(name, shape, dtype))
            self.hT32 = sb("hT32", [128, 8, S], F32)
            self.hTb = sb("hTb", [128, 8, S], BF16)
            self.vec = sb("vec_sb", [128, NV], F32)
            self.dv = sb("dv", [128, 64], F32)
            self.dv2 = sb("dv2", [128, 64], F32)
            self.cf = sb("cf_sb", [128, NCF], F32)
            self.identb = sb("identb", [128, 128], BF16)
            self.onesb = sb("onesb", [128, 128], BF16)
            self.causb = sb("causb", [128, 2, 256], BF16)
            self.e8b = sb("e8b", [8, 8, 128], BF16)
            self.cpow = sb("cpow", [128, 2], F32)
            self.wring = sb("wring", [128, self.NSLOT, self.SLOT_ELEMS], BF16)
            self.wring_i = Ring(self.NSLOT)
            self.zring, self.tring, self.ybank = Ring(3), Ring(4), Ring(2)
            self.arena = sb("arena", [128, self.ARENA_BYTES // 4], F32)
            self.ps = [es.enter_context(nc.psum_tensor("ps%d" % i, [128, 512], F32)) for i in range(8)]

            P.same_engine_sync = True
            self.pending_tail = None
            self.setup()
            for ph in self.phases:
                kind, li = ph
                P.phase_barrier()
                if kind == "attn":
                    self.attn(li)
                elif kind == "lru":
                    self.lru(li)
                elif kind == "ffn":
                    self.ffn(li)
            if self.pending_tail is not None:
                self.zip_run([self.pending_tail])
                self.pending_tail = None
            ov = self.outT.rearrange("(c p) t -> p c t", p=128)
            toks = []
            for t in range(4):
                toks.append(P.op("sp", lambda e, t=t: e.dma_start(out=ov[:, :, t * 512:(t + 1) * 512], in_=self.hT32[:, :, t * 512:(t + 1) * 512]),
                                 reads=[("h32", c, t) for c in range(8)], dma="dout%d" % t))
            P.final_waits("sp", toks)
            P.emit(nc, es)
        return nc

    def setup(self):
        P = self.P
        h32keys = [("h32", c, t) for c in range(8) for t in range(4)]
        hbkeys = [("hb", c, t) for c in range(8) for t in range(4)]
        P.op("sp", lambda e: e.dma_start(out=self.vec[:], in_=self.vecs_d), writes=["cst"], dma="dc")
        P.op("sp", lambda e: e.dma_start(out=self.cf[:], in_=self.cf_d), writes=["cst"], dma="dc")
        P.op("pool", lambda e: e.dma_start(out=self.e8b[:].rearrange("p a b -> p (a b)"), in_=self.e8_d), writes=["cb"], dma="dg0")
        xv = self.xT.rearrange("(c p) t -> p c t", p=128)
        for t in range(4):
            P.op("sp", lambda e, t=t: e.dma_start(out=self.hT32[:, :, t * 512:(t + 1) * 512], in_=xv[:, :, t * 512:(t + 1) * 512]),
                 writes=[("h32", c, t) for c in range(8)], dma="dx%d" % t)
        for t in range(4):
            for c in range(8):
                tsl = slice(t * 512, (t + 1) * 512)
                if c % 2 == 0:
                    P.op("dve", lambda e, c=c, tsl=tsl: e.tensor_copy(self.hTb[:, c, tsl], self.hT32[:, c, tsl]),
                         reads=[("h32", c, t)], writes=[("hb", c, t)])
                else:
                    P.op("act", lambda e, c=c, tsl=tsl: e.copy(self.hTb[:, c, tsl], self.hT32[:, c, tsl]),
                         reads=[("h32", c, t)], writes=[("hb", c, t)])
        P.op("dve", lambda e: e.tensor_copy(self.identb[:], self.cf[:, C_ID:C_ID + 128]), reads=["cst"], writes=["cb"])
        P.op("dve", lambda e: e.tensor_copy(self.causb[:].rearrange("p a b -> p (a b)"), self.cf[:, C_CAUS:C_CAUS + 512]),
             reads=["cst"], writes=["cb"])
        P.op("dve", lambda e: e.memset(self.onesb[:], 1.0), writes=["cb"])
        P.op("dve", lambda e: e.memset(self.cpow[:, 0:1], -0.5), writes=["cb"])
        P.op("dve", lambda e: e.memset(self.cpow[:, 1:2], 0.5), writes=["cb"])
        lam = self.vec[:, V_LAM:V_LAM + 16]
        e_, z_, z2_, q_ = (self.dv2[:, i * 16:(i + 1) * 16] for i in range(4))
        self.act(e_, lam, AF.Exp, ["cst"], ["dv"], scale=-1.0)
        self.ts(z_, e_, 2.0, None, ALU.add, None, ["dv"], ["dv"])
        P.op("dve", lambda e: e.reciprocal(z_, z_), reads=["dv"], writes=["dv"])
        self.tt(z_, z_, e_, ALU.mult, ["dv"], ["dv"])
        self.tt(z2_, z_, z_, ALU.mult, ["dv"], ["dv"])
        self.ts(q_, z2_, 1.0 / 13.0, None, ALU.mult, None, ["dv"], ["dv"])
        for cc in (1.0 / 11, 1.0 / 9, 1.0 / 7, 1.0 / 5, 1.0 / 3):
            self.stt(q_, q_, cc, z2_, ALU.add, ALU.mult, ["dv"], ["dv"])
        self.stt(q_, q_, 1.0, z_, ALU.add, ALU.mult, ["dv"], ["dv"])
        self.ts(self.dv[:, 16:32], q_, -8.0, None, ALU.mult, None, ["dv"], ["dv"])
        self.ts(self.dv[:, 0:16], q_, -16.0, None, ALU.mult, None, ["dv"], ["dv"])
        self.ts(self.dv[:, 32:48], self.vec[:, V_BA:V_BA + 16], 0.5, None, ALU.mult, None, ["cst", "dv"], ["dv"])
        self.ts(self.dv[:, 48:64], self.vec[:, V_BX:V_BX + 16], 0.5, None, ALU.mult, None, ["cst", "dv"], ["dv"])

    @staticmethod
    def zip_run(gens, weights=None):
        gens = list(gens)
        weights = list(weights) if weights else [1] * len(gens)
        alive = [True] * len(gens)
        while any(alive):
            for i, g in enumerate(gens):
                for _ in range(weights[i]):
                    if not alive[i]:
                        break
                    try:
                        next(g)
                    except StopIteration:
                        alive[i] = False

    def out_proj_ln(self, tts, nk, wsrc, rhs_fn, rhs_keys_fn, ln_col, tmp_off):
        P = self.P
        zb = [self.view(tmp_off + i * 1024, [128, 512], BF16) for i in range(3)]
        zq = [self.view(tmp_off + 3072 + i * 1024, [128, 512], BF16) for i in range(3)]
        o = tmp_off + 6144
        mean = [self.view(o + i * 2048, [128, 512], F32) for i in range(2)]
        var = [self.view(o + 4096 + i * 2048, [128, 512], F32) for i in range(2)]
        tmp = [self.view(o + 8192 + i * 2048, [128, 512], F32) for i in range(4)]
        zring, tring = self.zring, self.tring
        sbank = [(6, 7), (2, 3)]
        ybank = self.ybank

        def mloop():
            pending = []

            def flush():
                for (m, ti, zi) in pending:
                    b1, b2 = sbank[ti]
                    self.mm(self.ps[b1][:], self.onesb[:], zb[zi][:], m == 0, m == 7, [("zb", zi), "cb"], self.bk(b1), inc=True)
                    self.mm(self.ps[b2][:], self.onesb[:], zq[zi][:], m == 0, m == 7, [("zq", zi), "cb"], self.bk(b2), inc=True)
                pending.clear()

            for m in range(8):
                w, wkey = self.wload(wsrc(m), nk * 128)
                for ti, tt in enumerate(tts):
                    yb = 4 + ybank.next()
                    tsl = slice(tt * 512, (tt + 1) * 512)
                    for k in range(nk):
                        self.mm(self.ps[yb][:], w[:, k * 128:(k + 1) * 128], rhs_fn(k, tt), k == 0, k == nk - 1,
                                [wkey] + rhs_keys_fn(k, tt), [("ps", yb)])
                    flush()
                    hk = ("h32", m, tt)
                    self.stt(self.hT32[:, m, tsl], self.hT32[:, m, tsl], ALPHA, self.ps[yb][:], ALU.mult, ALU.add,
                             [hk, ("ps", yb)], [hk])
                    zi = zring.next()
                    self.act(zb[zi][:], self.hT32[:, m, tsl], AF.Copy, [hk], [("zb", zi)])
                    self.act(zq[zi][:], self.hT32[:, m, tsl], AF.Square, [hk], [("zq", zi)])
                    pending.append((m, ti, zi))
                    yield
            flush()

        def tail(ti, tt):
            b1, b2 = sbank[ti]
            tsl = slice(tt * 512, (tt + 1) * 512)
            mk, vk = ("lnmean", ti), ("lnvar", ti)
            self.act(mean[ti][:], self.ps[b1][:], AF.Identity, self.bk(b1), [mk], scale=1.0 / D)
            yield
            self.act(var[ti][:], self.ps[b2][:], AF.Identity, self.bk(b2), [vk], scale=1.0 / D, bias=EPS)
            yield
            ti_ = tring.next()
            self.tt(tmp[ti_][:], mean[ti][:], mean[ti][:], ALU.mult, [mk], [("lntmp", ti_)])
            yield
            self.tt(var[ti][:], var[ti][:], tmp[ti_][:], ALU.subtract, [vk, ("lntmp", ti_)], [vk])
            yield
            self.act(var[ti][:], var[ti][:], AF.Sqrt, [vk], [vk])
            yield
            P.op("dve", lambda e: e.reciprocal(var[ti][:], var[ti][:]), reads=[vk], writes=[vk])
            yield
            self.tt(mean[ti][:], mean[ti][:], var[ti][:], ALU.mult, [mk, vk], [mk])
            for m in range(8):
                yield
                hk = ("h32", m, tt)
                ti_ = tring.next()
                tk = ("lntmp", ti_)
                self.tt(tmp[ti_][:], self.hT32[:, m, tsl], var[ti][:], ALU.mult, [hk, vk], [tk])
                yield
                self.tt(tmp[ti_][:], tmp[ti_][:], mean[ti][:], ALU.subtract, [tk, mk], [tk])
                g = self.vcol(V_LNG + ln_col * 8 + m)
                b = self.vcol(V_LNB + ln_col * 8 + m)
                self.act(self.hT32[:, m, tsl], tmp[ti_][:], AF.Identity, [tk, "cst"], [hk], scale=g, bias=b)
                self.act(self.hTb[:, m, tsl], tmp[ti_][:], AF.Identity, [tk, "cst"], [("hb", m, tt)], scale=g, bias=b)

        def tails():
            gens = [tail(ti, tt) for ti, tt in enumerate(tts)]
            while gens:
                for g_ in list(gens):
                    try:
                        next(g_)
                        yield
                    except StopIteration:
                        gens.remove(g_)

        return mloop(), tails()

    def mixer_out(self, nk, wsrc, rhs_fn, rhs_keys_fn, ln_col, tmp_off):
        m0, t0 = self.out_proj_ln([0, 1], nk, wsrc, rhs_fn, rhs_keys_fn, ln_col, tmp_off)
        m1, t1 = self.out_proj_ln([2, 3], nk, wsrc, rhs_fn, rhs_keys_fn, ln_col, tmp_off)
        self.zip_run([m0])
        self.zip_run([t0, m1], [3, 1])
        self.P.snapshot_early()
        self.pending_tail = t1

    def ffn(self, li):
        P = self.P
        ACT_OFF = 0
        SG_OFF = 45056
        TMP_OFF = 49152
        actT = self.view(ACT_OFF, [128, NF, 1024], BF16)
        sg = [self.view(SG_OFF + i * 2048, [128, 512], F32) for i in range(2)]
        sgr = Ring(2)
        gb, ub = Ring(2), Ring(2)

        def inproj(tts):
            for f in range(NF):
                w, wkey = self.wload(self.fwin[li, f], 2048)
                for ti, tt in enumerate(tts):
                    g = gb.next()
                    u = 2 + ub.next()
                    tsl = slice(tt * 512, (tt + 1) * 512)
                    for k in range(8):
                        self.mm(self.ps[g][:], w[:, k * 128:(k + 1) * 128], self.hTb[:, k, tsl], k == 0, k == 7,
                                [wkey, ("hb", k, tt)], [("ps", g)])
                    for k in range(8):
                        self.mm(self.ps[u][:], w[:, 1024 + k * 128:1024 + (k + 1) * 128], self.hTb[:, k, tsl], k == 0, k == 7,
                                [wkey, ("hb", k, tt)], [("ps", u)])
                    si = sgr.next()
                    self.act(sg[si][:], self.ps[g][:], AF.Silu, [("ps", g)], [("sg", si)])
                    self.tt(actT[:, f, ti * 512:(ti + 1) * 512], sg[si][:], self.ps[u][:], ALU.mult,
                            [("sg", si), ("ps", u)], [("actT", f, ti)])
                    yield

        parts = []
        for grp in range(2):
            tts = [2 * grp, 2 * grp + 1]
            parts.append(self.out_proj_ln(tts, NF, lambda m: self.fwout[li, m],
                                          lambda k, tt: actT[:, k, (tt % 2) * 512:(tt % 2 + 1) * 512],
                                          lambda k, tt: [("actT", k, tt % 2)], li * 2 + 1, TMP_OFF))
        P.default_early = True
        tail, self.pending_tail = self.pending_tail, None
        if tail is not None:
            self.zip_run([tail, inproj([0, 1])], [2, 1])
        else:
            self.zip_run([inproj([0, 1])])
        P.default_early = False
        P.phase_barrier()
        self.zip_run([parts[0][0]])
        self.zip_run([parts[0][1], inproj([2, 3])], [2, 1])
        self.zip_run([parts[1][0]])
        P.snapshot_early()
        self.pending_tail = parts[1][1]

    def attn(self, li):
        P = self.P
        j = li // 2
        QT = [self.view(0 + i * 12288, [128, S], BF16) for i in range(2)]
        KT = [self.view(4096 + i * 12288, [128, S], BF16) for i in range(2)]
        VV = [self.view(8192 + i * 12288, [128, 16, 128], BF16) for i in range(2)]
        OT_OFF = 24576
        oT = self.view(OT_OFF, [128, 8, S], BF16)
        o = OT_OFF + 32768
        PT = [self.view(o + i * 512, [128, 256], BF16) for i in range(4)]
        o += 4 * 512
        lsT = self.view(o, [8, 2, S], BF16)
        o += 8192
        gm = self.view(o, [128, 128], F32); o += 512
        cmp_ = self.view(o, [128, 1024], F32); o += 4096
        rank = self.view(o, [128, 128], F32); o += 512
        lsel = [self.view(o + i * 512, [128, 128], F32) for i in range(2)]; o += 1024
        ksum = self.view(o, [128, 8], F32); o += 32
        kmT = [self.view(o + i * 32, [128, 8], BF16) for i in range(2)]; o += 64
        rden = [self.view(o + i * 1024, [128, 256], F32) for i in range(2)]; o += 2048
        assert o <= self.ARENA_BYTES
        TMP_OFF = 57344
        projb = Ring(2)
        ptr = Ring(4)
        sbk = Ring(2)
        rdr = Ring(2)
        past = self.cf[:, C_PAST:C_PAST + 128]

        def proj_g(h):
            hb = h % 2
            w, wkey = self.wload(self.wqkv[j, h], 3072)
            for tt in range(4):
                b = projb.next()
                tsl = slice(tt * 512, (tt + 1) * 512)
                for k in range(8):
                    self.mm(self.ps[b][:], w[:, k * 128:(k + 1) * 128], self.hTb[:, k, tsl], k == 0, k == 7,
                            [wkey, ("hb", k, tt)], [("ps", b)])
                self.act(QT[hb][:, tsl], self.ps[b][:], AF.Copy, [("ps", b)], [("QT", hb, tt)])
                yield
            for tt in range(4):
                b = projb.next()
                tsl = slice(tt * 512, (tt + 1) * 512)
                for k in range(8):
                    self.mm(self.ps[b][:], w[:, 1024 + k * 128:1024 + (k + 1) * 128], self.hTb[:, k, tsl], k == 0, k == 7,
                            [wkey, ("hb", k, tt)], [("ps", b)])
                P.op("dve", lambda e, b=b, tt=tt: e.tensor_reduce(
                    out=ksum[:, 2 * tt:2 * tt + 2], in_=self.ps[b][:].rearrange("p (a b) -> p a b", b=256),
                    axis=AX.X, op=ALU.add), reads=[], writes=[("ksum", tt), ("ps", b)])
                self.act(KT[hb][:, tsl], self.ps[b][:], AF.Copy, [], [("KT", hb, tt), ("ps", b)])
                yield
            self.ts(kmT[hb][:], ksum[:], 1.0 / 256, None, ALU.mult, None, [("ksum", t) for t in range(4)], [("kmT", hb)], ss=True)
            for g4 in range(4):
                b = projb.next()
                for t4 in range(4):
                    t16 = g4 * 4 + t4
                    for k in range(8):
                        self.mm(self.ps[b][:, t4 * 128:(t4 + 1) * 128], self.hTb[:, k, t16 * 128:(t16 + 1) * 128],
                                w[:, 2048 + k * 128:2048 + (k + 1) * 128], k == 0, k == 7,
                                [wkey, ("hb", k, t16 // 4)], [("ps", b)])
                P.op("dve", lambda e, b=b, g4=g4: e.tensor_copy(
                    VV[hb][:, g4 * 4:(g4 + 1) * 4, :].rearrange("p a b -> p (a b)"), self.ps[b][:]),
                    reads=[("ps", b)], writes=[("V", hb, g4)])
                yield

        def proj(h):
            for _ in proj_g(h):
                pass

        def gate_a(h):
            hb = h % 2
            for t in range(16):
                self.mm(self.ps[2][:, t * 8:(t + 1) * 8], QT[hb][:, t * 128:(t + 1) * 128], kmT[hb][:], True, True,
                        [("QT", hb, t // 4), ("kmT", hb)], [("ps", 2)], inc=(t == 15))
            self.tt(gm[:], self.ps[2][:, 0:128], past, ALU.add, [("ps", 2), "cst"], ["gm"], ss=True)
            g3 = gm[:].rearrange("p (t n) -> p t n", n=8)
            in0 = g3.unsqueeze(2).broadcast_to([128, 16, 8, 8])
            in1 = g3.unsqueeze(3).broadcast_to([128, 16, 8, 8])
            self.tt(cmp_[:].rearrange("p (t n m) -> p t n m", n=8, m=8), in0, in1, ALU.is_gt, ["gm"], ["cmp"], ss=True)
            P.op("dve", lambda e: e.tensor_reduce(out=rank[:], in_=cmp_[:].rearrange("p (a m) -> p a m", m=8),
                                                  axis=AX.X, op=ALU.add), reads=["cmp"], writes=["rank"], ss=True)
            self.ts(lsel[hb][:], rank[:], 2.5, NEG, ALU.is_gt, ALU.mult, ["rank"], [("lsel", hb)], ss=True)

        def gate_b(h):
            hb = h % 2
            for g4 in range(4):
                for t4 in range(4):
                    t = g4 * 4 + t4
                    P.op("pe", lambda e, t=t, t4=t4: e.transpose(self.ps[3][0:8, t4 * 128:(t4 + 1) * 128],
                                                                 lsel[hb][:, t * 8:(t + 1) * 8], self.cf[:, C_ID:C_ID + 128]),
                         reads=[("lsel", hb), "cst"], writes=[("ps", 3)], inc=(t4 == 3))
                self.act(lsT[0:8, hb, g4 * 512:(g4 + 1) * 512], self.ps[3][0:8, :], AF.Copy, [("ps", 3)], [("lsT", hb, g4)])

        def attention(h):
            hb = h % 2
            for qb in range(8):
                ob = 6 + qb % 2
                qsl = slice(qb * 256, (qb + 1) * 256)
                nkb = qb + 1
                pend = None

                def pv(kb, pslots, first, last):
                    for jj in range(2):
                        jt = 2 * kb + jj
                        st, sp_ = first and jj == 0, last and jj == 1
                        self.mm(self.ps[ob][:, 0:256], VV[hb][:, jt, :], PT[pslots[jj]][:], st, sp_,
                                [("V", hb, jt // 4), ("PT", pslots[jj])], [("ps", ob)], inc=True, sgc=True)
                        self.mm(self.ps[ob][:, 256:512], self.onesb[:], PT[pslots[jj]][:], False, sp_,
                                [("PT", pslots[jj]), "cb"], [("ps", ob)], inc=True, sgc=True)

                for kb in range(nkb):
                    sb_ = 4 + sbk.next()
                    slots = []
                    for jj in range(2):
                        jt = 2 * kb + jj
                        csl = slice(jj * 256, (jj + 1) * 256)
                        nomask = kb < qb and qb <= 3
                        self.mm(self.ps[sb_][:, csl], KT[hb][:, jt * 128:(jt + 1) * 128], QT[hb][:, qsl], True, nomask,
                                [("KT", hb, jt // 4), ("QT", hb, qb // 2)], [("ps", sb_)], inc=nomask)
                        if nomask:
                            pass
                        elif kb < qb:
                            self.mm(self.ps[sb_][:, csl], self.e8b[0:8, kb, :], lsT[0:8, hb, qsl], False, True,
                                    ["cb", ("lsT", hb, qb // 2)], [("ps", sb_)], inc=True)
                        else:
                            self.mm(self.ps[sb_][:, csl], self.identb[:], self.causb[:, jj, :], False, True,
                                    ["cb"], [("ps", sb_)], inc=True)
                    for jj in range(2):
                        jt = 2 * kb + jj
                        csl = slice(jj * 256, (jj + 1) * 256)
                        pi = ptr.next()
                        slots.append(pi)
                        r = jt - 2 * qb - 1 + 16
                        bias = self.cf[:, C_BIAS + h * 17 + r:C_BIAS + h * 17 + r + 1]
                        self.act(PT[pi][:], self.ps[sb_][:, csl], AF.Exp, [("ps", sb_), "cst"], [("PT", pi)],
                                 bias=bias, scale=SCALE)
                    if pend is not None:
                        pv(pend[0], pend[1], pend[0] == 0, False)
                    pend = (kb, slots)
                pv(pend[0], pend[1], pend[0] == 0, True)
                ri = rdr.next()
                P.op("dve", lambda e, ri=ri, ob=ob: e.reciprocal(rden[ri][:], self.ps[ob][:, 256:512]),
                     reads=[("ps", ob)], writes=[("rden", ri)])
                self.tt(oT[:, h, qsl], self.ps[ob][:, 0:256], rden[ri][:], ALU.mult,
                        [("ps", ob), ("rden", ri)], [("oT", h, qb // 2)], ss=True)

        P.default_early = True
        tail, self.pending_tail = self.pending_tail, None
        if tail is not None:
            self.zip_run([tail])
        proj(0)
        P.default_early = False
        P.phase_barrier()
        gate_a(0)
        gate_b(0)
        for h in range(NH):
            if h + 1 < NH:
                proj(h + 1)
                gate_a(h + 1)
            attention(h)
            if h + 1 < NH:
                gate_b(h + 1)
        self.mixer_out(8, lambda m: self.wo[j, m], lambda k, tt: oT[:, k, tt * 512:(tt + 1) * 512],
                       lambda k, tt: [("oT", k, tt)], li * 2, TMP_OFF)

    def lru(self, li):
        P = self.P
        j = li // 2
        if self.pending_tail is not None:
            tail, self.pending_tail = self.pending_tail, None
            P.default_early = True
            self.zip_run([tail])
            P.default_early = False
            P.phase_barrier()
        HS = 1024
        mT = self.view(0, [128, 8, S], BF16)
        o = 32768
        A2 = [self.view(o + i * 2048, [128, 512], F32) for i in range(2)]; o += 4096
        OM2 = [self.view(o + i * 2048, [128, 512], F32) for i in range(2)]; o += 4096
        IX2 = [self.view(o + i * 2048, [128, 512], F32) for i in range(2)]; o += 4096
        GT2 = [self.view(o + i * 2048, [128, 512], F32) for i in range(2)]; o += 4096
        xpad = [self.view(o + i * 2064, [128, 516], F32) for i in range(3)]; o += 6192
        xc = [self.view(o + i * 2048, [128, 512], F32) for i in range(4)]; o += 8192
        xcb = [self.view(o + i * 1024, [128, 512], BF16) for i in range(2)]; o += 2048
        t1 = [self.view(o + i * 2048, [128, 512], F32) for i in range(6)]; o += 12288
        U = [self.view(o + i * 2048, [128, 512], F32) for i in range(2)]; o += 4096
        wab = self.view(o, [128, 8, 128], BF16); o += 2048
        wxb = self.view(o, [128, 8, 128], BF16); o += 2048
        carry = self.view(o, [128, 2], F32); o += 8
        assert o <= self.ARENA_BYTES, o
        TMP_OFF = 57344
        P.op("pool", lambda e: e.dma_start(out=wab[:].rearrange("p a b -> p (a b)"), in_=self.lwa[j], max_dma_last_dim=4096),
             writes=["wab"], dma="dga")
        P.op("pool", lambda e: e.dma_start(out=wxb[:].rearrange("p a b -> p (a b)"), in_=self.lwx[j], max_dma_last_dim=4096),
             writes=["wxb"], dma="dgb")
        xbk, ybk = Ring(2), Ring(2)
        xpr, xcr, xcbr, t1r, ur = Ring(3), Ring(4), Ring(2), Ring(6), Ring(2)
        allk = lambda n: [(n, t) for t in range(2)]
        st = {}
        cur = {"w": None, "prev_xp": None}

        def consts(c):
            return dict(cw=[self.vcol(V_CW + (j * 4 + tap) * 8 + c) for tap in range(4)],
                        cb=self.vcol(V_CB + j * 8 + c),
                        hcl=self.dv[:, 16 + j * 8 + c:16 + j * 8 + c + 1],
                        hba=self.dv[:, 32 + j * 8 + c:32 + j * 8 + c + 1],
                        hbx=self.dv[:, 48 + j * 8 + c:48 + j * 8 + c + 1])

        def s1(n, c, tt, xi, pxi):
            w, wkey = cur["w"]
            k_ = consts(c)
            tsl = slice(tt * 512, (tt + 1) * 512)
            xb_ = xbk.next()
            yb_ = 2 + ybk.next()
            ci = xcr.next()
            bi = xcbr.next()
            rb, ib = 4 + 2 * (n % 2), 5 + 2 * (n % 2)
            st[n] = dict(yb=yb_, ci=ci, rb=rb, ib=ib)
            for k in range(8):
                self.mm(self.ps[xb_][:], w[:, k * 128:(k + 1) * 128], self.hTb[:, k, tsl], k == 0, k == 7,
                        [wkey, ("hb", k, tt)], [("ps", xb_)])
            for k in range(8):
                self.mm(self.ps[yb_][:], w[:, 1024 + k * 128:1024 + (k + 1) * 128], self.hTb[:, k, tsl], k == 0, k == 7,
                        [wkey, ("hb", k, tt)], [("ps", yb_)])
            yield
            xk = ("xpad", xi)
            self.act(xpad[xi][:, 3:515], self.ps[xb_][:], AF.Copy, [("ps", xb_)], [xk])
            ck = ("xc", ci)
            self.act(xc[ci][:], self.ps[xb_][:], AF.Identity, [("ps", xb_), "cst"], [ck], scale=k_["cw"][3], bias=k_["cb"])
            yield
            if tt == 0:
                P.op("dve", lambda e: e.memset(xpad[xi][:, 0:3], 0.0), writes=[xk])
            else:
                P.op("dve", lambda e: e.tensor_copy(xpad[xi][:, 0:3], xpad[pxi][:, 512:515]),
                     reads=[("xpad", pxi)], writes=[xk])
            for tap in (2, 1, 0):
                yield
                self.stt(xc[ci][:], xpad[xi][:, tap:tap + 512], k_["cw"][tap], xc[ci][:], ALU.mult, ALU.add,
                         [xk, ck, "cst"], [ck])
            yield
            self.act(xcb[bi][:], xc[ci][:], AF.Copy, [ck], [("xcb", bi)])
            yield
            self.mm(self.ps[rb][:], wab[:, c, :], xcb[bi][:], True, True, ["wab", ("xcb", bi)], [("ps", rb)])
            self.mm(self.ps[ib][:], wxb[:, c, :], xcb[bi][:], True, True, ["wxb", ("xcb", bi)], [("ps", ib)])

        def s2(n, c, tt):
            k_ = consts(c)
            d = st.pop(n)
            yb_, ci, rb, ib = d["yb"], d["ci"], d["rb"], d["ib"]
            ck = ("xc", ci)
            tl = n % 2
            lsl = slice(0, 512)
            A_, OM, IX, GT = A2[tl], OM2[tl], IX2[tl], GT2[tl]
            ta, tb, tg, ua = t1r.next(), t1r.next(), t1r.next(), ur.next()
            tk, tbk, tgk, uk = ("t1", ta), ("t1", tb), ("t1", tg), ("U", ua)
            self.act(t1[ta][:], self.ps[rb][:], AF.Tanh, [("ps", rb), "dv"], [tk], scale=0.5, bias=k_["hba"])
            yield
            self.act(U[ua][:], t1[ta][:], AF.Identity, [tk, "dv"], [uk], scale=k_["hcl"], bias=k_["hcl"])
            yield
            self.act(t1[ta][:], U[ua][:], AF.Identity, [uk], [tk], scale=1.0 / 24.0, bias=1.0 / 6.0)
            yield
            self.act(t1[tb][:], self.ps[ib][:], AF.Tanh, [("ps", ib), "dv"], [tbk], scale=0.5, bias=k_["hbx"])
            yield
            self.act(GT[:, lsl], self.ps[yb_][:], AF.Gelu_apprx_tanh, [("ps", yb_)], [("GT", tl)])
            for cc in (None, 0.5, 1.0):
                yield
                if cc is None:
                    self.tt(t1[ta][:], t1[ta][:], U[ua][:], ALU.mult, [tk, uk], [tk])
                else:
                    self.stt(t1[ta][:], t1[ta][:], cc, U[ua][:], ALU.add, ALU.mult, [tk, uk], [tk])
            yield
            self.stt(U[ua][:], t1[ta][:], 2.0, t1[ta][:], ALU.add, ALU.mult, [tk], [uk])
            yield
            self.stt(IX[:, lsl], t1[tb][:], 1.0, xc[ci][:], ALU.add, ALU.mult, [tbk, ck], [("IX", tl)])
            yield
            self.act(A_[:, lsl], t1[ta][:], AF.Identity, [tk], [("A", tl)], bias=1.0)
            yield
            self.act(OM[:, lsl], U[ua][:], AF.Relu, [uk], [("OM", tl)], scale=-1.0)

        def s3(n, c, tt):
            tl = n % 2
            A_, OM, IX, GT = A2[tl], OM2[tl], IX2[tl], GT2[tl]
            tsl = slice(tt * 512, (tt + 1) * 512)
            self.act(OM[:], OM[:], AF.Sqrt, [("OM", tl)], [("OM", tl)])
            yield
            yield
            self.stt(IX[:], OM[:], 0.5, IX[:], ALU.mult, ALU.mult, [("OM", tl), ("IX", tl)], [("IX", tl)])
            yield
            yield
            init = 0.0 if tt == 0 else carry[:, 0:1]
            P.op("dve", lambda e: e.tensor_tensor_scan(out=OM[:], data0=A_[:], data1=IX[:], initial=init,
                                                       op0=ALU.mult, op1=ALU.add),
                 reads=[("A", tl), ("IX", tl), "carry"], writes=[("OM", tl)])
            if tt < 3:
                P.op("pool", lambda e: e.tensor_copy(carry[:, 0:1], OM[:, 511:512]), reads=[("OM", tl)], writes=["carry"])
            self.tt(mT[:, c, tsl], OM[:], GT[:], ALU.mult, [("OM", tl), ("GT", tl)], [("mT", c, tt)], eng="pool")

        def zip_run(gens):
            gens = list(gens)
            while gens:
                for g in list(gens):
                    try:
                        next(g)
                    except StopIteration:
                        gens.remove(g)

        tiles = [(c, tt) for c in range(8) for tt in range(4)]
        NT_ = len(tiles)
        for n in range(NT_ + 2):
            gens = []
            if n < NT_:
                c, tt = tiles[n]
                if tt == 0:
                    cur["w"] = self.wload(self.lwin[j, c], 2048)
                xi = xpr.next()
                pxi = cur["prev_xp"]
                cur["prev_xp"] = xi
                gens.append(s1(n, c, tt, xi, pxi))
            if 1 <= n <= NT_:
                gens.append(s2(n - 1, *tiles[n - 1]))
            if n >= 2:
                gens.append(s3(n - 2, *tiles[n - 2]))
            zip_run(gens)
        if self.dbg == "lru_mT":
            for c in range(8):
                tok = P.op("pool", lambda e, c=c: e.dma_start(out=self.dbgT[c * 128:(c + 1) * 128, :], in_=mT[:, c, :], max_dma_last_dim=4096),
                           reads=[("mT", c, t) for t in range(4)], dma="ddbg")
            P.final_waits("pool", [tok])
            return
        self.mixer_out(8, lambda m: self.lwout[j, m], lambda k, tt: mT[:, k, tt * 512:(tt + 1) * 512],
                       lambda k, tt: [("mT", k, tt)], li * 2, TMP_OFF)


def _consts():
    cf = np.zeros((128, NCF), np.float32)
    cf[:, C_ID:C_ID + 128] = np.eye(128, dtype=np.float32)
    p = np.arange(128, dtype=np.float64)
    for h in range(NH):
        slope = 2.0 ** (-8.0 * (h + 1) / NH)
        for r in range(17):
            cf[:, C_BIAS + h * 17 + r] = slope * (p + 128.0 * (r - 16))
    past = np.zeros((16, 8), np.float32)
    for t in range(16):
        for n in range(8):
            if n >= t // 2:
                past[t, n] = -1e30
    cf[:, C_PAST:C_PAST + 128] = past.reshape(1, 128)
    q = np.arange(256)
    for half in range(2):
        k = half * 128 + np.arange(128)
        cf[:, C_CAUS + half * 256:C_CAUS + (half + 1) * 256] = np.where(q[None, :] >= k[:, None], 0.0, NEG)
    e8 = np.zeros((8, 8, 128), np.float32)
    for kb in range(8):
        e8[kb, kb, :] = 1.0
    return cf, e8.reshape(8, 1024)


def _prep_weights(inp):
    f = lambda a: np.ascontiguousarray(a, dtype=np.float32)
    out = {}
    w = inp["attn_w_qkv"].reshape(2, 8, 128, 3, NH, 128)
    out["wqkv"] = f(w.transpose(0, 4, 2, 3, 1, 5).reshape(2, NH, 128, 3072))
    w = inp["attn_w_o"].reshape(2, 8, 128, 8, 128)
    out["wo"] = f(w.transpose(0, 3, 2, 1, 4).reshape(2, 8, 128, 1024))
    w = inp["lru_w_in"].reshape(2, 8, 128, 2, 8, 128)
    out["lwin"] = f(w.transpose(0, 4, 2, 3, 1, 5).reshape(2, 8, 128, 2048))
    w = inp["lru_w_out"].reshape(2, 8, 128, 8, 128)
    out["lwout"] = f(w.transpose(0, 3, 2, 1, 4).reshape(2, 8, 128, 1024))
    out["lwa"] = f(inp["lru_w_a"].transpose(0, 2, 1, 3).reshape(2, 128, 1024))
    out["lwx"] = f(inp["lru_w_x"].transpose(0, 2, 1, 3).reshape(2, 128, 1024))
    w = inp["ffn_w_in"].reshape(DEPTH, 8, 128, 2, NF, 128)
    out["fwin"] = f(w.transpose(0, 4, 2, 3, 1, 5).reshape(DEPTH, NF, 128, 2048))
    w = inp["ffn_w_out"].reshape(DEPTH, NF, 128, 8, 128)
    out["fwout"] = f(w.transpose(0, 3, 2, 1, 4).reshape(DEPTH, 8, 128, FF))
    vecs = np.zeros((128, NV), np.float32)
    pc = lambda a: a.reshape(a.shape[:-1] + (8, 128))
    vecs[:, V_LNG:V_LNG + 64] = pc(inp["ln_g"]).transpose(3, 0, 1, 2).reshape(128, 64)
    vecs[:, V_LNB:V_LNB + 64] = pc(inp["ln_b"]).transpose(3, 0, 1, 2).reshape(128, 64)
    vecs[:, V_CW:V_CW + 64] = pc(inp["lru_conv_w"]).transpose(3, 0, 1, 2).reshape(128, 64)
    vecs[:, V_CB:V_CB + 16] = pc(inp["lru_conv_b"]).transpose(2, 0, 1).reshape(128, 16)
    vecs[:, V_BA:V_BA + 16] = pc(inp["lru_b_a"]).transpose(2, 0, 1).reshape(128, 16)
    vecs[:, V_BX:V_BX + 16] = pc(inp["lru_b_x"]).transpose(2, 0, 1).reshape(128, 16)
    vecs[:, V_LAM:V_LAM + 16] = pc(inp["lru_lambda"]).transpose(2, 0, 1).reshape(128, 16)
    out["vecs"] = vecs
    out["cf"], out["e8"] = _consts()
    return out


ALL_PHASES = [("attn", 0), ("ffn", 0), ("lru", 1), ("ffn", 1), ("attn", 2), ("ffn", 2), ("lru", 3), ("ffn", 3)]
_NC_CACHE = {}


def run(inputs, phases=None, trace=False, dbg=None):
    phases = ALL_PHASES if phases is None else phases
    key = (tuple(phases), dbg)
    if key not in _NC_CACHE:
        _NC_CACHE[key] = K(phases, dbg).build()
    nc = _NC_CACHE[key]
    inp = {k: np.asarray(v) for k, v in inputs.items()}
    shared = _prep_weights(inp)
    x = np.asarray(inp["x"], dtype=np.float32)
    in_maps = []
    for b in range(8):
        m = dict(shared)
        m["xT"] = np.ascontiguousarray(x[b].T)
        in_maps.append(m)
    res = run_bass_kernel_spmd(nc, in_maps, core_ids=list(range(8)), trace=trace)
    out = np.stack([np.ascontiguousarray(r["outT"].T) for r in res.results], axis=0).astype(np.float32)
    if dbg:
        out = np.stack([np.ascontiguousarray(r["dbgT"].T) for r in res.results], axis=0).astype(np.float32)
    return out, res


def kernel(**inputs):
    out, _ = run(inputs)
    return out
```
